# Optimizing a Trainium2 kernel written in Bass

```python
import jax, jax.numpy as jnp
from jax import lax
import numpy as np

D_MODEL = 2048
BATCH = 4
SEQ = 4096
DEPTH = 2

N_A_LAYERS = DEPTH // 2
N_B_LAYERS = DEPTH - N_A_LAYERS
N_DENSE_LAYERS = (DEPTH + 1) // 2
N_MOE_LAYERS = DEPTH // 2

GLA_HEADS = 4
GLA_DK = D_MODEL // 2
GLA_DV = D_MODEL
GLA_DK_HEAD = GLA_DK // GLA_HEADS
GLA_DV_HEAD = GLA_DV // GLA_HEADS
GLA_GATE_RANK = 16
GLA_TAU = 16.0
GLA_CHUNK = 64

MOBA_HEADS = 16
MOBA_KV_HEADS = 4
MOBA_HEAD_DIM = D_MODEL // MOBA_HEADS
MOBA_BLOCK = 256
MOBA_TOPK = 3
MOBA_QGROUP = 128
MOBA_GROUPS_PER_STEP = 32

FFN_DENSE = 5504
N_EXPERTS = 8
TOP_K = 2
FFN_EXPERT = 7168
MOE_GROUP = 1024

LN_EPS = 1e-5
RMS_EPS = 1e-6
DEEPNORM_ALPHA = (2 * DEPTH) ** 0.25
DEEPNORM_BETA = (8 * DEPTH) ** -0.25

kernel_name = "yoco_gla_moba_moe_deepnorm"

F32 = jnp.float32


def layer_norm(x, g, b):
    xf = x.astype(F32)
    mu = jnp.mean(xf, -1, keepdims=True)
    var = jnp.mean(jnp.square(xf - mu), -1, keepdims=True)
    return ((xf - mu) * lax.rsqrt(var + LN_EPS) * g + b).astype(x.dtype)


def post_norm(x, y, g, b):
    return layer_norm(DEEPNORM_ALPHA * x + y, g, b)


def group_rows(seg, num_segments, group, multiple):
    n_asg = seg.shape[0]
    cap = -(-(n_asg + num_segments * group) // multiple) * multiple
    counts = jax.ops.segment_sum(jnp.ones_like(seg), seg, num_segments=num_segments)
    padded = (counts + group - 1) // group * group
    pad_end = jnp.cumsum(padded)
    pad_start = pad_end - padded
    start = jnp.cumsum(counts) - counts
    order = jnp.argsort(seg)
    sorted_seg = seg[order]
    dest_sorted = pad_start[sorted_seg] + jnp.arange(n_asg, dtype=seg.dtype) - start[sorted_seg]
    dest = jnp.zeros_like(seg).at[order].set(dest_sorted)
    group_start = jnp.arange(cap // group, dtype=seg.dtype) * group
    group_seg = jnp.minimum(jnp.searchsorted(pad_end, group_start, side="right"), num_segments - 1)
    group_used = group_start < pad_end[-1]
    return dest, group_seg, group_used, cap


def gla_mixer(h, w_in, w_gate_up, b_gate, g_norm, w_o):
    bsz, s, _ = h.shape
    n_chunk = -(-s // GLA_CHUNK)
    pad = n_chunk * GLA_CHUNK - s
    proj = h @ w_in
    q, k, v, r, z = jnp.split(
        proj, [GLA_DK, 2 * GLA_DK, 2 * GLA_DK + GLA_DV, 2 * GLA_DK + 2 * GLA_DV], axis=-1)
    log_a = jax.nn.log_sigmoid((z @ w_gate_up + b_gate).astype(F32)) / GLA_TAU

    def chunked(t, hd):
        t = jnp.pad(t.astype(F32), ((0, 0), (0, pad), (0, 0)))
        return t.reshape(bsz, n_chunk, GLA_CHUNK, GLA_HEADS, hd)

    q_c = chunked(q, GLA_DK_HEAD) * (GLA_DK_HEAD ** -0.5)
    k_c = chunked(k, GLA_DK_HEAD)
    v_c = chunked(v, GLA_DV_HEAD)
    b_cum = jnp.cumsum(chunked(log_a, GLA_DK_HEAD), axis=2)
    b_last = b_cum[:, :, -1:]
    q_dec = q_c * jnp.exp(b_cum)
    k_inv = k_c * jnp.exp(-b_cum)
    k_end = k_c * jnp.exp(b_last - b_cum)
    causal = jnp.tril(jnp.ones((GLA_CHUNK, GLA_CHUNK), bool))
    att = jnp.einsum("bncht,bnmht->bnhcm", q_dec, k_inv)
    att = jnp.where(causal, att, 0.0)
    o_intra = jnp.einsum("bnhcm,bnmhv->bnchv", att, v_c)
    decay = jnp.exp(b_last[:, :, 0])

    def scan_step(state, xs):
        q_n, k_n, v_n, d_n = xs
        o_n = jnp.einsum("bcht,bhtv->bchv", q_n, state)
        state = d_n[..., None] * state + jnp.einsum("bcht,bchv->bhtv", k_n, v_n)
        return state, o_n

    mv = lambda t: jnp.moveaxis(t, 1, 0)
    state0 = jnp.zeros((bsz, GLA_HEADS, GLA_DK_HEAD, GLA_DV_HEAD), F32)
    _, o_inter = lax.scan(scan_step, state0, (mv(q_dec), mv(k_end), mv(v_c), mv(decay)))
    o = (o_intra + jnp.moveaxis(o_inter, 0, 1)).reshape(bsz, n_chunk * GLA_CHUNK, GLA_HEADS, GLA_DV_HEAD)[:, :s]
    o = o * lax.rsqrt(jnp.mean(o * o, -1, keepdims=True) + RMS_EPS) * g_norm
    o = (o.reshape(bsz, s, GLA_DV) * jax.nn.silu(r.astype(F32))).astype(h.dtype)
    return o @ w_o


def moba_shared_kv(h, w_kv):
    bsz, s, _ = h.shape
    n_blk = -(-s // MOBA_BLOCK)
    pad = n_blk * MOBA_BLOCK - s
    kv = jnp.pad(h @ w_kv, ((0, 0), (0, pad), (0, 0)))
    kv = kv.reshape(bsz, n_blk, MOBA_BLOCK, 2, MOBA_KV_HEADS, MOBA_HEAD_DIM)
    k_blk = kv[:, :, :, 0].transpose(0, 3, 1, 2, 4)
    v_blk = kv[:, :, :, 1].transpose(0, 3, 1, 2, 4)
    count = jnp.clip(s - jnp.arange(n_blk) * MOBA_BLOCK, 1, MOBA_BLOCK).astype(F32)
    k_mean = k_blk.astype(F32).sum(3) / count[None, None, :, None]
    return k_blk, v_blk, k_mean


def grouped_block_attention(q_rows, k_seg, v_seg, group_seg, group_used):
    cap, hd = q_rows.shape
    n_steps = cap // (MOBA_QGROUP * MOBA_GROUPS_PER_STEP)
    qs = q_rows.reshape(n_steps, MOBA_GROUPS_PER_STEP, MOBA_QGROUP, hd)
    segs = group_seg.reshape(n_steps, MOBA_GROUPS_PER_STEP)
    used = group_used.reshape(n_steps, MOBA_GROUPS_PER_STEP)

    def step(args):
        qg, sg, ug = args

        def compute():
            kg = k_seg[sg].astype(F32)
            vg = v_seg[sg].astype(F32)
            sc = jnp.einsum("pqd,pkd->pqk", qg.astype(F32), kg)
            m = sc.max(-1)
            p = jnp.exp(sc - m[..., None])
            return jnp.einsum("pqk,pkd->pqd", p, vg), m, p.sum(-1)

        def skip():
            zr = jnp.zeros(qg.shape[:-1], F32)
            return jnp.zeros(qg.shape, F32), zr, zr

        return lax.cond(jnp.any(ug), compute, skip)

    o, m, l = lax.map(step, (qs, segs, used))
    return o.reshape(cap, hd), m.reshape(cap), l.reshape(cap)


def moba_mixer(h, w_q, w_o, k_blk, v_blk, k_mean):
    bsz, s, _ = h.shape
    n_blk = k_blk.shape[2]
    s_pad = n_blk * MOBA_BLOCK
    rep = MOBA_HEADS // MOBA_KV_HEADS
    hd = MOBA_HEAD_DIM
    q = (h @ w_q).reshape(bsz, s, MOBA_KV_HEADS, rep, hd) * (hd ** -0.5)

    gate = jnp.einsum("bsgrd,bgnd->bsgrn", q.astype(F32), k_mean)
    q_blk = jnp.arange(s) // MOBA_BLOCK
    past = jnp.arange(n_blk)[None, :] < q_blk[:, None]
    gate = jnp.where(past[None, :, None, None, :], gate, -jnp.inf)
    top_k = min(MOBA_TOPK, n_blk)
    _, sel = lax.top_k(gate, top_k)
    valid = sel < q_blk[None, :, None, None, None]

    q_pad = jnp.pad(q, ((0, 0), (0, s_pad - s), (0, 0), (0, 0), (0, 0)))
    q_pad = q_pad.reshape(bsz, n_blk, MOBA_BLOCK, MOBA_KV_HEADS, rep, hd)
    s_own = jnp.einsum("bnqgrd,bgnkd->bnqgrk", q_pad.astype(F32), k_blk.astype(F32))
    causal = jnp.tril(jnp.ones((MOBA_BLOCK, MOBA_BLOCK), bool))
    s_own = jnp.where(causal[None, None, :, None, None, :], s_own, -jnp.inf)
    m_own = s_own.max(-1)
    p_own = jnp.exp(s_own - m_own[..., None])
    l_own = p_own.sum(-1).reshape(bsz, s_pad, MOBA_KV_HEADS, rep)[:, :s]
    o_own = jnp.einsum("bnqgrk,bgnkd->bnqgrd", p_own, v_blk.astype(F32))
    o_own = o_own.reshape(bsz, s_pad, MOBA_KV_HEADS, rep, hd)[:, :s]
    m_own = m_own.reshape(bsz, s_pad, MOBA_KV_HEADS, rep)[:, :s]

    n_seg = bsz * MOBA_KV_HEADS * n_blk
    b_idx = jnp.arange(bsz)[:, None, None, None, None]
    g_idx = jnp.arange(MOBA_KV_HEADS)[None, None, :, None, None]
    seg = ((b_idx * MOBA_KV_HEADS + g_idx) * n_blk + sel).reshape(-1).astype(jnp.int32)
    dest, group_seg, group_used, cap = group_rows(
        seg, n_seg, MOBA_QGROUP, MOBA_QGROUP * MOBA_GROUPS_PER_STEP)
    q_rows = jnp.zeros((cap, hd), q.dtype).at[dest].set(jnp.repeat(q.reshape(-1, hd), top_k, axis=0))
    k_seg = k_blk.reshape(n_seg, MOBA_BLOCK, hd)
    v_seg = v_blk.reshape(n_seg, MOBA_BLOCK, hd)
    o_rows, m_rows, l_rows = grouped_block_attention(q_rows, k_seg, v_seg, group_seg, group_used)
    sel_shape = (bsz, s, MOBA_KV_HEADS, rep, top_k)
    o_sel = o_rows[dest].reshape(sel_shape + (hd,))
    m_sel = jnp.where(valid, m_rows[dest].reshape(sel_shape), -jnp.inf)
    l_sel = l_rows[dest].reshape(sel_shape)

    m_tot = jnp.maximum(m_own, m_sel.max(-1))
    w_own = jnp.exp(m_own - m_tot)
    w_sel = jnp.exp(m_sel - m_tot[..., None])
    num = o_own * w_own[..., None] + jnp.einsum("bsgrt,bsgrtd->bsgrd", w_sel, o_sel)
    den = l_own * w_own + (l_sel * w_sel).sum(-1)
    out = (num / den[..., None]).astype(h.dtype).reshape(bsz, s, MOBA_HEADS * hd)
    return out @ w_o


def swiglu(h, w_gu, w_down):
    g, u = jnp.split(h @ w_gu, 2, axis=-1)
    return (jax.nn.silu(g) * u) @ w_down


def moe_swiglu(h, w_router, w_gu, w_down):
    bsz, s, d = h.shape
    xt = h.reshape(-1, d)
    n_tok = xt.shape[0]
    logits = (xt @ w_router).astype(F32)
    top_val, top_idx = lax.top_k(logits, TOP_K)
    gates = jax.nn.softmax(top_val, axis=-1)
    seg = top_idx.reshape(-1).astype(jnp.int32)
    dest, group_seg, group_used, cap = group_rows(seg, N_EXPERTS, MOE_GROUP, MOE_GROUP)
    rows = jnp.zeros((cap, d), xt.dtype).at[dest].set(jnp.repeat(xt, TOP_K, axis=0))
    out_dtype = jnp.result_type(xt, w_down)

    def step(args):
        xg, e, used = args

        def compute():
            g, u = jnp.split(xg @ w_gu[e], 2, axis=-1)
            return ((jax.nn.silu(g) * u) @ w_down[e]).astype(out_dtype)

        return lax.cond(used, compute, lambda: jnp.zeros(xg.shape, out_dtype))

    y_rows = lax.map(step, (rows.reshape(-1, MOE_GROUP, d), group_seg, group_used)).reshape(cap, d)
    y = (y_rows[dest].reshape(n_tok, TOP_K, d).astype(F32) * gates[..., None]).sum(1)
    return y.astype(h.dtype).reshape(bsz, s, d)


def setup_inputs(seed: int = 0) -> dict:
    key = jax.random.key(seed)
    ks = jax.random.split(key, 20)

    def dense(k, shape, fan_in, scale=1.0):
        return jax.random.normal(k, shape, F32) * (scale * fan_in ** -0.5)

    def gain(k, shape):
        return 1.0 + 0.02 * jax.random.normal(k, shape, F32)

    def bias(k, shape, scale=0.02):
        return scale * jax.random.normal(k, shape, F32)

    gla_in_width = 2 * GLA_DK + 2 * GLA_DV + GLA_GATE_RANK
    return {
        "x": jax.random.normal(ks[0], (BATCH, SEQ, D_MODEL), F32),
        "w_in_a": dense(ks[1], (N_A_LAYERS, D_MODEL, gla_in_width), D_MODEL),
        "w_gate_up_a": dense(ks[2], (N_A_LAYERS, GLA_GATE_RANK, GLA_DK), GLA_GATE_RANK),
        "b_gate_a": bias(ks[3], (N_A_LAYERS, GLA_DK), 0.1),
        "g_norm_a": gain(ks[4], (N_A_LAYERS, GLA_DV_HEAD)),
        "w_o_a": dense(ks[5], (N_A_LAYERS, GLA_DV, D_MODEL), GLA_DV, DEEPNORM_BETA),
        "w_kv_shared": dense(ks[6], (D_MODEL, 2 * MOBA_KV_HEADS * MOBA_HEAD_DIM), D_MODEL),
        "w_q_b": dense(ks[7], (N_B_LAYERS, D_MODEL, MOBA_HEADS * MOBA_HEAD_DIM), D_MODEL),
        "w_o_b": dense(ks[8], (N_B_LAYERS, MOBA_HEADS * MOBA_HEAD_DIM, D_MODEL), MOBA_HEADS * MOBA_HEAD_DIM, DEEPNORM_BETA),
        "ln_mix_g": gain(ks[9], (DEPTH, D_MODEL)),
        "ln_mix_b": bias(ks[10], (DEPTH, D_MODEL)),
        "w_gu_dense": dense(ks[11], (N_DENSE_LAYERS, D_MODEL, 2 * FFN_DENSE), D_MODEL),
        "w_down_dense": dense(ks[12], (N_DENSE_LAYERS, FFN_DENSE, D_MODEL), FFN_DENSE, DEEPNORM_BETA),
        "w_router": dense(ks[13], (N_MOE_LAYERS, D_MODEL, N_EXPERTS), D_MODEL),
        "w_gu_moe": dense(ks[14], (N_MOE_LAYERS, N_EXPERTS, D_MODEL, 2 * FFN_EXPERT), D_MODEL),
        "w_down_moe": dense(ks[15], (N_MOE_LAYERS, N_EXPERTS, FFN_EXPERT, D_MODEL), FFN_EXPERT, DEEPNORM_BETA),
        "ln_ffn_g": gain(ks[16], (DEPTH, D_MODEL)),
        "ln_ffn_b": bias(ks[17], (DEPTH, D_MODEL)),
    }


def reference(x, w_in_a, w_gate_up_a, b_gate_a, g_norm_a, w_o_a, w_kv_shared, w_q_b, w_o_b,
              ln_mix_g, ln_mix_b, w_gu_dense, w_down_dense, w_router, w_gu_moe, w_down_moe,
              ln_ffn_g, ln_ffn_b):
    h = x
    shared = None
    for i in range(DEPTH):
        if i < N_A_LAYERS:
            y = gla_mixer(h, w_in_a[i], w_gate_up_a[i], b_gate_a[i], g_norm_a[i], w_o_a[i])
        else:
            if shared is None:
                shared = moba_shared_kv(h, w_kv_shared)
            j = i - N_A_LAYERS
            y = moba_mixer(h, w_q_b[j], w_o_b[j], *shared)
        h = post_norm(h, y, ln_mix_g[i], ln_mix_b[i])
        if i % 2 == 0:
            y = swiglu(h, w_gu_dense[i // 2], w_down_dense[i // 2])
        else:
            y = moe_swiglu(h, w_router[i // 2], w_gu_moe[i // 2], w_down_moe[i // 2])
        h = post_norm(h, y, ln_ffn_g[i], ln_ffn_b[i])
    return h
```

```python
import contextlib
import math
import numpy as np
import concourse.bass as bass
import concourse.mybir as mybir
from concourse.bass_utils import run_bass_kernel_spmd

F32 = mybir.dt.float32
BF16 = mybir.dt.bfloat16
AF = mybir.ActivationFunctionType
ALU = mybir.AluOpType
AX = mybir.AxisListType

ENGS = ("pe", "act", "dve", "pool", "sp")

D = 2048
T = 2048
NKC = 16
G = 512
ALPHA = (2 * 2) ** 0.25
LN_EPS = 1e-5
RMS_EPS = 1e-6
F_DENSE = 5504
F_EXP = 7168
NEXP = 8


class Buf:
    __slots__ = ("name", "w", "r")

    def __init__(self, name=""):
        self.name = name
        self.w = None
        self.r = {}


class Prog:
    def __init__(self, nc, stack, n_dma_sems=16):
        self.nc = nc
        self.stack = stack
        self.q = {e: [] for e in ENGS}
        self.sems = {}
        self.cnt = {}
        for e in ENGS:
            self.sems["E" + e] = stack.enter_context(nc.semaphore("prog_" + e))
            self.cnt["E" + e] = 0
        self.dma_ring = {}
        for e in ("sp", "pool", "act"):
            keys = []
            for i in range(n_dma_sems):
                k = f"D{e}{i}"
                self.sems[k] = stack.enter_context(nc.semaphore(f"dma_{e}{i}"))
                self.cnt[k] = 0
                keys.append(k)
            self.dma_ring[e] = [keys, 0]
        self.nops = 0
        self.floor = {}
        self.floor_pending = set()

    def init_arena(self, nwords=44032):
        self.big = self.stack.enter_context(self.nc.sbuf_tensor("arena", [128, nwords], F32))
        self.a_words = nwords
        self.a_off = 0
        self.a_mark = 0

    def sb(self, name, shape, dt):
        esz = 2 if dt == BF16 else 4
        n = 1
        for s_ in shape[1:]:
            n *= s_
        words = (n * esz + 3) // 4
        words = (words + 7) // 8 * 8
        assert self.a_off + words <= self.a_words, f"SBUF arena overflow at {name}: {self.a_off}+{words}"
        v = self.big[0:shape[0], self.a_off:self.a_off + words]
        self.a_off += words
        if dt != F32:
            v = v.bitcast(dt)
        v = v[:, 0:n]
        if len(shape) == 3:
            v = v.rearrange("p (a b) -> p a b", a=shape[1])
        elif len(shape) == 4:
            v = v.rearrange("p (a b c) -> p a b c", a=shape[1], b=shape[2])
        return v

    def mark(self):
        self.a_mark = self.a_off

    def reset(self):
        self.a_off = self.a_mark
        self.floor = dict(self.cnt)
        self.floor_pending = set(ENGS)

    def ps(self, name, shape, dt=F32):
        return self.stack.enter_context(self.nc.psum_tensor(name, list(shape), dt))

    def _collect(self, eng, reads, writes, is_dma):
        waits = {}
        if eng in self.floor_pending:
            self.floor_pending.discard(eng)
            waits.update({k: v for k, v in self.floor.items() if v > 0})

        def need(sem, val, src, war=False):
            if src == eng and not is_dma:
                if eng == "pe" or war:
                    return
            if val > waits.get(sem, 0):
                waits[sem] = val

        for b in reads:
            if b.w is not None:
                need(*b.w)
        for b in writes:
            if b.w is not None:
                need(*b.w)
            for sem, (val, src) in b.r.items():
                need(sem, val, src, war=True)
        return waits

    def _commit(self, reads, writes, sem, val, eng):
        for b in reads:
            old = b.r.get(sem)
            if old is None or old[0] < val:
                b.r[sem] = (val, eng)
        for b in writes:
            b.w = (sem, val, eng)
            b.r = {}

    def op(self, eng, fn, reads=(), writes=()):
        waits = self._collect(eng, reads, writes, False)
        sem = "E" + eng
        self.cnt[sem] += 1
        val = self.cnt[sem]
        self.q[eng].append((waits, fn, sem, 1))
        self._commit(reads, writes, sem, val, eng)
        self.nops += 1

    def dma(self, eng, fn, reads=(), writes=()):
        waits = self._collect(eng, reads, writes, True)
        ring = self.dma_ring[eng]
        sem = ring[0][ring[1] % len(ring[0])]
        ring[1] += 1
        prev = self.cnt[sem]
        if prev > waits.get(sem, 0):
            waits[sem] = prev
        self.cnt[sem] += 16
        val = self.cnt[sem]
        self.q[eng].append((waits, fn, sem, 16))
        self._commit(reads, writes, sem, val, eng)
        self.nops += 1

    def emit(self):
        nc = self.nc
        engobj = {"pe": "tensor", "act": "scalar", "dve": "vector", "pool": "gpsimd", "sp": "sync"}
        final = dict(self.cnt)
        with nc.Block() as block:
            for e in ENGS:
                items = self.q[e]

                def body(eo, items=items, e=e):
                    waited = {}
                    for waits, fn, sem, amt in items:
                        for s, v in waits.items():
                            if waited.get(s, 0) < v:
                                eo.wait_ge(self.sems[s], v)
                                waited[s] = v
                        ins = fn(eo)
                        ins.then_inc(self.sems[sem], amt)
                    if e == "sp":
                        for s, v in final.items():
                            if v > 0 and waited.get(s, 0) < v:
                                eo.wait_ge(self.sems[s], v)

                getattr(block, engobj[e])(body)


def bcast_rows(ap_row, n):
    return bass.AP(ap_row.tensor, ap_row.offset, [[0, 128], [1, n]])


class Ctx:
    pass


def setup_ctx(nc, p, cst):
    c = Ctx()
    c.nc, c.p = nc, p
    c.PB = [p.ps(f"pb{i}", [128, 512]) for i in range(8)]
    c.PBb = [Buf(f"pb{i}") for i in range(8)]
    c.id32 = p.sb("id32", [128, 128], F32)
    c.idb = p.sb("idb", [128, 128], BF16)
    c.id_b = Buf("ident")
    p.dma("sp", lambda e: e.dma_start(out=c.id32[:], in_=cst["ident"]), writes=[c.id_b])
    p.dma("pool", lambda e: e.dma_start(out=c.idb[:], in_=cst["ident"]), writes=[c.id_b])
    c.rr = 0
    return c


def evac(c, out, in_, reads, writes, eng=None):
    p = c.p
    if eng is None:
        eng = ("act", "dve")[c.rr % 2]
        c.rr += 1
    if eng == "act":
        p.op("act", lambda e: e.activation(out=out, in_=in_, func=AF.Copy), reads=reads, writes=writes)
    else:
        p.op("dve", lambda e: e.tensor_copy(out=out, in_=in_), reads=reads, writes=writes)


def transpose_tile32(c, src, srcB, dstb, dstB, col0, banks, dst32=None, dst32B=None):
    p = c.p
    for b in range(4):
        bk = banks[b]
        for j in range(4):
            kc = 4 * b + j
            p.op("pe", lambda e, bk=bk, j=j, kc=kc: e.transpose(
                out=c.PB[bk][:, j * 128:(j + 1) * 128], in_=src[:, kc * 128:(kc + 1) * 128], identity=c.id32[:]),
                reads=[srcB, c.id_b], writes=[c.PBb[bk]])
        pv = c.PB[bk][:, :].rearrange("p (a b) -> p a b", a=4)
        evac(c, dstb[:, 4 * b:4 * b + 4, col0:col0 + 128], pv, [c.PBb[bk]], [dstB], eng="act")
        if dst32 is not None:
            evac(c, dst32[:, 4 * b:4 * b + 4, col0:col0 + 128], pv, [c.PBb[bk]], dst32B[4 * b:4 * b + 4], eng="act")


def load_w_block(c, dst, dstB, w, col0, ncols, nk=NKC):
    src = w[:, col0:col0 + ncols].rearrange("(kc p) c -> p kc c", p=128)
    c.p.dma("pool", lambda e: e.dma_start(out=dst, in_=src), writes=[dstB])


def ln_epilogue(c, banks, resid, residB, out_dram, gbc, bbc, lnB, W):
    p = c.p
    st, mv, rs, B2 = W["st"], W["mv"], W["rs"], W["B2"]
    t, tB = resid, residB
    for s in range(4):
        sl = slice(s * 512, (s + 1) * 512)
        p.op("dve", lambda e, s=s, sl=sl: e.scalar_tensor_tensor(
            out=t[:, sl], in0=t[:, sl], scalar=ALPHA, in1=c.PB[banks[s]][:, :], op0=ALU.mult, op1=ALU.add),
            reads=[tB, c.PBb[banks[s]]], writes=[tB])
    for s in range(4):
        p.op("dve", lambda e, s=s: e.bn_stats(out=st[:, s * 6:(s + 1) * 6], in_=t[:, s * 512:(s + 1) * 512]),
             reads=[tB], writes=[B2])
    p.op("dve", lambda e: e.bn_aggr(out=mv, in_=st), reads=[B2], writes=[B2])
    p.op("act", lambda e: e.activation(out=rs, in_=mv[:, 1:2], func=AF.Sqrt, bias=LN_EPS, scale=1.0),
         reads=[B2], writes=[B2])
    p.op("dve", lambda e: e.reciprocal(out=rs, in_=rs), reads=[B2], writes=[B2])
    p.op("dve", lambda e: e.tensor_scalar(out=t, in0=t, scalar1=mv[:, 0:1], scalar2=rs[:, 0:1],
                                          op0=ALU.subtract, op1=ALU.mult), reads=[tB, B2], writes=[tB])
    p.op("pool", lambda e: e.tensor_tensor(out=t, in0=t, in1=gbc, op=ALU.mult), reads=[tB, lnB], writes=[tB])
    p.op("pool", lambda e: e.tensor_tensor(out=t, in0=t, in1=bbc, op=ALU.add), reads=[tB, lnB], writes=[tB])
    p.dma("sp", lambda e: e.dma_start(out=out_dram, in_=t), reads=[tB], writes=[W["outB"]])


def ln_work(p, tag, outB):
    return {"st": p.sb(f"lnst{tag}", [128, 24], F32),
            "mv": p.sb(f"lnmv{tag}", [128, 2], F32), "rs": p.sb(f"lnrs{tag}", [128, 1], F32), "B2": Buf(),
            "outB": outB}


def load_ln(c, g_row, b_row, gbc, bbc, lnB):
    c.p.dma("sp", lambda e: e.dma_start(out=gbc, in_=bcast_rows(g_row, D)), writes=[lnB])
    c.p.dma("sp", lambda e: e.dma_start(out=bbc, in_=bcast_rows(b_row, D)), writes=[lnB])


def gla_phase(c, x_prev, x_own, w_in, w_gate_up, b_gate, g_norm, cmask_d, oT_d):
    p = c.p
    p.reset()
    xin = p.sb("xin", [128, D], F32); xinB = Buf()
    xT = p.sb("xT", [128, NKC, G], BF16); xTB = Buf()
    wblk = [p.sb(f"wblk{i}", [128, NKC, 256], BF16) for i in range(2)]; wblkB = [Buf(), Buf()]
    wz = p.sb("wz", [128, NKC, 16], BF16); wzB = Buf()
    wgu = p.sb("wgu", [17, 1024], BF16); wguB = Buf()
    zT = p.sb("zT", [17, G], BF16); zTB = Buf()
    cs = p.sb("cs", [128, 8, G], F32); csB = [Buf() for _ in range(8)]
    dec = p.sb("dec", [128, 8, 4], F32)
    e1 = [p.sb(f"e1_{i}", [128, G], F32) for i in range(2)]; e1B = [Buf(), Buf()]
    ones = p.sb("ones", [128, 128], F32); onesB = Buf()
    qdT = p.sb("qdT", [128, 8, G], BF16); qdB = [Buf() for _ in range(8)]
    kiT = p.sb("kiT", [128, 8, G], BF16); kiB = [Buf() for _ in range(8)]
    keT = p.sb("keT", [128, 8, G], BF16); keB = [Buf() for _ in range(8)]
    v = p.sb("v", [128, 4, D], BF16); vB = [Buf() for _ in range(4)]
    sr = p.sb("sr", [128, 4, D], BF16); srB = [Buf() for _ in range(4)]
    gnbc = p.sb("gnbc", [128, D], F32); gnB = Buf()
    stmp = [p.sb(f"stmp{i}", [128, 256], F32) for i in range(2)]; stmpB = [Buf(), Buf()]
    S = p.sb("S", [128, 8, 512], F32); SB = [Buf() for _ in range(8)]
    Sb = p.sb("Sb", [128, 8, 512], BF16); SbB = [Buf() for _ in range(8)]
    ke = p.sb("ke", [128, 1024], BF16); keTB = Buf()
    attT = p.sb("attT", [128, 4, 128], BF16); attB = Buf()
    cmask = p.sb("cmask", [128, 4, 128], F32); cmB = Buf()
    junk = p.sb("junk", [128, 512], F32); junkB = Buf()
    ss = p.sb("ss", [128, 4], F32); rst = p.sb("rst", [128, 4], F32); ssB = Buf()
    ofin = p.sb("ofin", [128, D], BF16); ofinB = Buf()
    ofT = p.sb("ofT", [128, NKC, 128], BF16); ofTB = Buf()
    outB = Buf()

    p.dma("pool", lambda e: e.dma_start(out=wz, in_=w_in[:, 6144:6160].rearrange("(kc p) c -> p kc c", p=128)), writes=[wzB])
    p.dma("pool", lambda e: e.dma_start(out=wgu[0:16, :], in_=w_gate_up), writes=[wguB])
    p.dma("pool", lambda e: e.dma_start(out=wgu[16:17, :], in_=b_gate), writes=[wguB])
    for h in range(4):
        p.dma("sp", lambda e, h=h: e.dma_start(out=gnbc[:, h * 512:(h + 1) * 512], in_=bcast_rows(g_norm, 512)), writes=[gnB])
    for h in range(4):
        p.dma("sp", lambda e, h=h: e.dma_start(out=cmask[:, h, :], in_=cmask_d), writes=[cmB])
    p.op("pool", lambda e: e.memset(ones, 1.0), writes=[onesB])
    p.op("pool", lambda e: e.memset(zT, 1.0), writes=[zTB])
    p.op("pool", lambda e: e.memset(S.rearrange("p a b -> p (a b)"), 0.0), writes=SB)
    p.op("pool", lambda e: e.memset(Sb.rearrange("p a b -> p (a b)"), 0.0), writes=SbB)

    LNSC = math.log(256 ** -0.5)
    wi = [0]

    def wload(col0):
        i = wi[0] % 2
        wi[0] += 1
        load_w_block(c, wblk[i], wblkB[i], w_in, col0, 256)
        return i

    for g in range(8):
        own = g >= 4
        xsrc = x_own if own else x_prev
        g4 = g % 4
        for t in range(4):
            r0 = g4 * G + t * 128
            p.dma("sp", lambda e, r0=r0, xsrc=xsrc: e.dma_start(out=xin, in_=xsrc[r0:r0 + 128, :]), writes=[xinB])
            transpose_tile32(c, xin, xinB, xT, xTB, t * 128, (4, 5, 6, 7))
        for kc in range(NKC):
            p.op("pe", lambda e, kc=kc: e.matmul(c.PB[0][0:16, :], lhsT=wz[:, kc, :], rhs=xT[:, kc, :], start=(kc == 0), stop=(kc == NKC - 1)),
                 reads=[wzB, xTB], writes=[c.PBb[0]])
        evac(c, zT[0:16, :], c.PB[0][0:16, :], [c.PBb[0]], [zTB], eng="act")
        for dk in range(8):
            bk = 1 + dk % 2
            p.op("pe", lambda e, dk=dk, bk=bk: e.matmul(c.PB[bk][:, :], lhsT=wgu[:, dk * 128:(dk + 1) * 128], rhs=zT, start=True, stop=True),
                 reads=[wguB, zTB], writes=[c.PBb[bk]])
            a, b = e1[0], e1[1]
            p.op("act", lambda e, bk=bk, a=a: e.activation(out=a, in_=c.PB[bk][:, :], func=AF.Exp, scale=-1.0), reads=[c.PBb[bk]], writes=[e1B[0]])
            p.op("act", lambda e, a=a, b=b: e.activation(out=b, in_=a, func=AF.Ln, bias=1.0, scale=1.0), reads=[e1B[0]], writes=[e1B[1]])
            for ch in range(4):
                p.op("dve", lambda e, dk=dk, ch=ch, b=b: e.tensor_tensor_scan(
                    out=cs[:, dk, ch * 128:(ch + 1) * 128], data0=ones, data1=b[:, ch * 128:(ch + 1) * 128], initial=0.0,
                    op0=ALU.mult, op1=ALU.add), reads=[onesB, e1B[1]], writes=[csB[dk]])
            p.op("act", lambda e, dk=dk: e.activation(
                out=dec[:, dk, :], in_=cs[:, dk, :].rearrange("p (a b) -> p a b", a=4)[:, :, 127], func=AF.Exp, scale=-1.0 / 16.0),
                reads=[csB[dk]], writes=[csB[dk]])
        kinds = [("k", b) for b in range(4)] + ([("q", b) for b in range(4)] if own else [])
        kinds += [("v", b) for b in range(8)] + ([("r", b) for b in range(8)] if own else [])
        base = {"q": 0, "k": 1024, "v": 2048, "r": 4096}
        nxt = wload(base[kinds[0][0]] + kinds[0][1] * 256)
        for bi, (kind, b) in enumerate(kinds):
            cur = nxt
            if bi + 1 < len(kinds):
                nxt = wload(base[kinds[bi + 1][0]] + kinds[bi + 1][1] * 256)
            W = wblk[cur]
            if kind in ("q", "k"):
                for j in range(2):
                    cc = 2 * b + j
                    bk = (2 * bi + j) % 4
                    for kc in range(NKC):
                        p.op("pe", lambda e, W=W, j=j, kc=kc, bk=bk: e.matmul(
                            c.PB[bk][:, :], lhsT=W[:, kc, j * 128:(j + 1) * 128], rhs=xT[:, kc, :], start=(kc == 0), stop=(kc == NKC - 1)),
                            reads=[wblkB[cur], xTB], writes=[c.PBb[bk]])
                    et = e1[0]
                    if kind == "q":
                        p.op("act", lambda e, cc=cc, et=et: e.activation(out=et, in_=cs[:, cc, :], func=AF.Exp, scale=-1.0 / 16.0, bias=LNSC),
                             reads=[csB[cc]], writes=[e1B[0]])
                        p.op("dve", lambda e, cc=cc, bk=bk, et=et: e.tensor_tensor(out=qdT[:, cc, :], in0=c.PB[bk][:, :], in1=et, op=ALU.mult),
                             reads=[c.PBb[bk], e1B[0]], writes=[qdB[cc]])
                    else:
                        p.op("act", lambda e, cc=cc, et=et: e.activation(out=et, in_=cs[:, cc, :], func=AF.Exp, scale=1.0 / 16.0),
                             reads=[csB[cc]], writes=[e1B[0]])
                        p.op("dve", lambda e, cc=cc, bk=bk, et=et: e.tensor_tensor(out=kiT[:, cc, :], in0=c.PB[bk][:, :], in1=et, op=ALU.mult),
                             reads=[c.PBb[bk], e1B[0]], writes=[kiB[cc]])
                        for ch in range(4):
                            sl = slice(ch * 128, (ch + 1) * 128)
                            p.op("dve", lambda e, cc=cc, bk=bk, et=et, ch=ch, sl=sl: e.scalar_tensor_tensor(
                                out=keT[:, cc, sl], in0=c.PB[bk][:, sl], scalar=dec[:, cc, ch:ch + 1], in1=et[:, sl], op0=ALU.mult, op1=ALU.mult),
                                reads=[c.PBb[bk], e1B[0], csB[cc]], writes=[keB[cc]])
            else:
                col0 = b * 256
                for t in range(4):
                    bk = 4 + (t // 2) + 2 * (bi % 2)
                    half = (t % 2) * 256
                    for kc in range(NKC):
                        p.op("pe", lambda e, W=W, t=t, kc=kc, bk=bk, half=half: e.matmul(
                            c.PB[bk][:, half:half + 256], lhsT=xT[:, kc, t * 128:(t + 1) * 128], rhs=W[:, kc, :], start=(kc == 0), stop=(kc == NKC - 1)),
                            reads=[wblkB[cur], xTB], writes=[c.PBb[bk]])
                    if kind == "v":
                        evac(c, v[:, t, col0:col0 + 256], c.PB[bk][:, half:half + 256], [c.PBb[bk]], [vB[t]], eng="act")
                    else:
                        si = t % 2
                        p.op("act", lambda e, bk=bk, half=half, si=si: e.activation(out=stmp[si], in_=c.PB[bk][:, half:half + 256], func=AF.Silu),
                             reads=[c.PBb[bk]], writes=[stmpB[si]])
                        p.op("pool", lambda e, t=t, col0=col0, si=si: e.tensor_tensor(out=sr[:, t, col0:col0 + 256], in0=stmp[si], in1=gnbc[:, col0:col0 + 256], op=ALU.mult),
                             reads=[stmpB[si], gnB], writes=[srB[t]])
        for ch in range(4):
            sl = slice(ch * 128, (ch + 1) * 128)
            if own:
                for h in range(4):
                    for tcl in range(2):
                        tc = 2 * h + tcl
                        p.op("pe", lambda e, h=h, tc=tc, tcl=tcl, sl=sl: e.matmul(
                            c.PB[4][:, h * 128:(h + 1) * 128], lhsT=kiT[:, tc, sl], rhs=qdT[:, tc, sl], start=(tcl == 0), stop=(tcl == 1)),
                            reads=[kiB[tc], qdB[tc]], writes=[c.PBb[4]])
                p.op("dve", lambda e: e.tensor_tensor(out=attT, in0=c.PB[4][:, :].rearrange("p (a b) -> p a b", a=4), in1=cmask, op=ALU.mult),
                     reads=[c.PBb[4], cmB], writes=[attB])
                for h in range(4):
                    p.op("pe", lambda e, h=h, ch=ch: e.matmul(c.PB[h][:, :], lhsT=attT[:, h, :], rhs=v[:, ch, h * 512:(h + 1) * 512], start=True, stop=False),
                         reads=[attB, vB[ch]], writes=[c.PBb[h]])
                    for tcl in range(2):
                        tc = 2 * h + tcl
                        p.op("pe", lambda e, h=h, tc=tc, tcl=tcl, sl=sl: e.matmul(c.PB[h][:, :], lhsT=qdT[:, tc, sl], rhs=Sb[:, tc, :], start=False, stop=(tcl == 1)),
                             reads=[qdB[tc], SbB[tc]], writes=[c.PBb[h]])
            pb7 = c.PB[7].bitcast(BF16)
            for tc in range(8):
                p.op("pe", lambda e, tc=tc, sl=sl: e.transpose(out=pb7[:, tc * 128:(tc + 1) * 128], in_=keT[:, tc, sl], identity=c.idb),
                     reads=[keB[tc], c.id_b], writes=[c.PBb[7]])
            evac(c, ke, pb7[:, 0:1024], [c.PBb[7]], [keTB], eng="act")
            for tc in range(8):
                h = tc // 2
                bk = 5 + tc % 2
                p.op("pe", lambda e, tc=tc, h=h, bk=bk, ch=ch: e.matmul(c.PB[bk][:, :], lhsT=ke[:, tc * 128:(tc + 1) * 128], rhs=v[:, ch, h * 512:(h + 1) * 512], start=True, stop=True),
                     reads=[keTB, vB[ch]], writes=[c.PBb[bk]])
                p.op("dve", lambda e, tc=tc, bk=bk, ch=ch: e.scalar_tensor_tensor(
                    out=S[:, tc, :], in0=S[:, tc, :], scalar=dec[:, tc, ch:ch + 1], in1=c.PB[bk][:, :], op0=ALU.mult, op1=ALU.add),
                    reads=[SB[tc], c.PBb[bk], csB[tc]], writes=[SB[tc]])
                p.op("pool", lambda e, tc=tc: e.tensor_copy(out=Sb[:, tc, :], in_=S[:, tc, :]), reads=[SB[tc]], writes=[SbB[tc]])
            if own:
                for h in range(4):
                    p.op("act", lambda e, h=h: e.activation(out=junk, in_=c.PB[h][:, :], func=AF.Square, accum_out=ss[:, h:h + 1]),
                         reads=[c.PBb[h]], writes=[junkB, ssB])
                p.op("act", lambda e: e.activation(out=rst, in_=ss, func=AF.Sqrt, scale=1.0 / 512.0, bias=RMS_EPS), reads=[ssB], writes=[ssB])
                p.op("dve", lambda e: e.reciprocal(out=rst, in_=rst), reads=[ssB], writes=[ssB])
                for h in range(4):
                    hs = slice(h * 512, (h + 1) * 512)
                    p.op("dve", lambda e, h=h, hs=hs, ch=ch: e.scalar_tensor_tensor(
                        out=ofin[:, hs], in0=c.PB[h][:, :], scalar=rst[:, h:h + 1], in1=sr[:, ch, hs], op0=ALU.mult, op1=ALU.mult),
                        reads=[c.PBb[h], ssB, srB[ch]], writes=[ofinB])
                for half in range(2):
                    bk = 5 + half
                    pbv = c.PB[bk].bitcast(BF16)
                    for j in range(8):
                        kc = half * 8 + j
                        p.op("pe", lambda e, pbv=pbv, j=j, kc=kc: e.transpose(out=pbv[:, j * 128:(j + 1) * 128], in_=ofin[:, kc * 128:(kc + 1) * 128], identity=c.idb),
                             reads=[ofinB, c.id_b], writes=[c.PBb[bk]])
                    evac(c, ofT[:, half * 8:half * 8 + 8, :], pbv[:, 0:1024].rearrange("p (a b) -> p a b", a=8), [c.PBb[bk]], [ofTB], eng="act")
                tile = g4 * 4 + ch
                p.dma("sp", lambda e, tile=tile: e.dma_start(out=oT_d[tile], in_=ofT), reads=[ofTB], writes=[outB])


def proj_ln_phase(c, aT_d, w, resid_d, g_row, b_row, out_d):
    p = c.p
    p.reset()
    W = p.sb("W", [128, NKC, D], BF16); WB = [Buf() for _ in range(4)]
    aT = [p.sb(f"aT{i}", [128, NKC, 128], BF16) for i in range(2)]; aTB = [Buf(), Buf()]
    res = [p.sb(f"res{i}", [128, D], F32) for i in range(2)]; resB = [Buf(), Buf()]
    gbc = p.sb("gbc", [128, D], F32); bbc = p.sb("bbc", [128, D], F32); lnB = Buf()
    outB = Buf()
    lw = [ln_work(p, i, outB) for i in range(2)]
    for s in range(4):
        load_w_block(c, W[:, :, s * 512:(s + 1) * 512], WB[s], w, s * 512, 512)
    load_ln(c, g_row, b_row, gbc, bbc, lnB)
    for t in range(T // 128):
        i = t % 2
        p.dma("sp", lambda e, t=t, i=i: e.dma_start(out=aT[i], in_=aT_d[t]), writes=[aTB[i]])
        p.dma("sp", lambda e, t=t, i=i: e.dma_start(out=res[i], in_=resid_d[t * 128:(t + 1) * 128, :]), writes=[resB[i]])
        banks = [4 * i + s for s in range(4)]
        for s in range(4):
            for kc in range(NKC):
                p.op("pe", lambda e, s=s, kc=kc, i=i, bk=banks[s]: e.matmul(
                    c.PB[bk][:, :], lhsT=aT[i][:, kc, :], rhs=W[:, kc, s * 512:(s + 1) * 512], start=(kc == 0), stop=(kc == NKC - 1)),
                    reads=[aTB[i], WB[s]], writes=[c.PBb[banks[s]]])
        ln_epilogue(c, banks, res[i], resB[i], out_d[t * 128:(t + 1) * 128, :], gbc, bbc, lnB, lw[i])


MOEDBG = 0


def ffn_phase(c, h_d, w_gu_list, w_d_list, F, g_row, b_row, out_d, w_router=None, esel_d=None):
    p = c.p
    p.reset()
    moe = w_router is not None
    nfc = F // 128
    hin = [p.sb("hin0", [128, D], F32)] * 2; hinB = [Buf()] * 2
    hT = p.sb("hT", [128, NKC, G], BF16); hTB = Buf()
    big32 = p.sb("big32", [128, NKC, G], F32); bigB = [Buf() for _ in range(NKC)]
    wg = [p.sb(f"wg{i}", [128, NKC, 128], BF16) for i in range(2)]; wgB = [Buf(), Buf()]
    wu = [p.sb(f"wu{i}", [128, NKC, 128], BF16) for i in range(2)]; wuB = [Buf(), Buf()]
    nh0 = (nfc + 1) // 2
    wd = [p.sb("wd0", [128, nh0, 128], BF16), p.sb("wd1", [128, nfc - nh0, 128], BF16)]; wdB = [Buf(), Buf()]
    actT = p.sb("actT", [128, nfc, G], BF16); actB = [Buf() for _ in range(nfc)]
    sg = [p.sb("sg0", [128, G], F32)] * 2; sgB = [Buf()] * 2
    gbc = p.sb("gbc", [128, D], F32); bbc = p.sb("bbc", [128, D], F32); lnB = Buf()
    outB = Buf()
    lw = [ln_work(p, 0, outB)]
    load_ln(c, g_row, b_row, gbc, bbc, lnB)
    if moe:
        wr = p.sb("wr", [128, NKC, NEXP], F32); wrB = Buf()
        esel = p.sb("esel", [8, NEXP, 128], F32); eselB = Buf()
        p.dma("sp", lambda e: e.dma_start(out=wr, in_=w_router.rearrange("(kc p) c -> p kc c", p=128)), writes=[wrB])
        p.dma("sp", lambda e: e.dma_start(out=esel, in_=esel_d), writes=[eselB])
        lg = p.sb("lg", [128, 8], F32); mx = p.sb("mx", [128, 8], F32); el = p.sb("el", [128, 8], F32)
        msk = p.sb("msk", [128, 8], F32); sm = p.sb("sm", [128, 4], F32); gts = p.sb("gts", [128, 8], F32); rB = Buf()
        gT = p.sb("gT", [8, G], F32); gTB = Buf()
        gb = p.sb("gb", [128, G], F32); gbB = Buf()
        tmp = [p.sb("tmp0", [128, G], F32)] * 2; tmpB = [Buf()] * 2
    n_exp = len(w_gu_list)
    for g in range(T // G):
        for t in range(4):
            i = t % 2
            r0 = g * G + t * 128
            p.dma("sp", lambda e, r0=r0, i=i: e.dma_start(out=hin[i], in_=h_d[r0:r0 + 128, :]), writes=[hinB[i]])
            transpose_tile32(c, hin[i], hinB[i], hT, hTB, t * 128, (4, 5, 6, 7), dst32=big32 if (moe and not (MOEDBG & 8)) else None,
                             dst32B=bigB)
        if moe:
            for t in range(4):
                if MOEDBG & 1:
                    p.op("dve", lambda e: e.tensor_copy(out=lg, in_=wr[:, 0, :]), reads=[wrB], writes=[rB])
                else:
                    for kc in range(NKC):
                        p.op("pe", lambda e, t=t, kc=kc: e.matmul(c.PB[0][:, 0:8], lhsT=big32[:, kc, t * 128:(t + 1) * 128], rhs=wr[:, kc, :],
                                                                  start=(kc == 0), stop=(kc == NKC - 1)),
                             reads=[bigB[kc], wrB], writes=[c.PBb[0]])
                    p.op("dve", lambda e: e.tensor_copy(out=lg, in_=c.PB[0][:, 0:8]), reads=[c.PBb[0]], writes=[rB])
                p.op("dve", lambda e: e.max(out=mx, in_=lg), reads=[rB], writes=[rB])
                p.op("dve", lambda e: e.tensor_scalar(out=sm[:, 0:1], in0=mx[:, 0:1], scalar1=-1.0, scalar2=None, op0=ALU.mult), reads=[rB], writes=[rB])
                p.op("act", lambda e: e.activation(out=el, in_=lg, func=AF.Exp, bias=sm[:, 0:1], scale=1.0), reads=[rB], writes=[rB])
                p.op("act", lambda e: e.activation(out=sm[:, 1:2], in_=mx[:, 1:2], func=AF.Exp, bias=sm[:, 0:1], scale=1.0), reads=[rB], writes=[rB])
                p.op("dve", lambda e: e.tensor_scalar(out=sm[:, 2:3], in0=sm[:, 1:2], scalar1=1.0, scalar2=None, op0=ALU.add), reads=[rB], writes=[rB])
                p.op("dve", lambda e: e.reciprocal(out=sm[:, 3:4], in_=sm[:, 2:3]), reads=[rB], writes=[rB])
                p.op("dve", lambda e: e.tensor_scalar(out=msk, in0=lg, scalar1=mx[:, 1:2], scalar2=None, op0=ALU.is_ge), reads=[rB], writes=[rB])
                p.op("dve", lambda e: e.scalar_tensor_tensor(out=gts, in0=el, scalar=sm[:, 3:4], in1=msk, op0=ALU.mult, op1=ALU.mult), reads=[rB], writes=[rB])
                if not (MOEDBG & 2):
                    p.op("pe", lambda e, t=t: e.transpose(out=c.PB[1][0:8, t * 128:(t + 1) * 128], in_=gts, identity=c.id32), reads=[rB, c.id_b], writes=[c.PBb[1]])
            if MOEDBG & 2:
                p.op("dve", lambda e: e.memset(gT, 0.5), reads=[rB], writes=[gTB])
            else:
                evac(c, gT, c.PB[1][0:8, :], [c.PBb[1]], [gTB], eng="dve")
        blk = [0]
        for ex in range(n_exp):
            w_gu, w_dn = w_gu_list[ex], w_d_list[ex]
            if moe and (MOEDBG & 4):
                p.op("dve", lambda e: e.memset(gb, 0.5), reads=[gTB], writes=[gbB])
            elif moe:
                p.op("pe", lambda e, ex=ex: e.matmul(c.PB[0][:, :], lhsT=esel[:, ex, :], rhs=gT, start=True, stop=True), reads=[eselB, gTB], writes=[c.PBb[0]])
                evac(c, gb, c.PB[0][:, :], [c.PBb[0]], [gbB], eng="act")
            def wl(j):
                i = blk[0] % 2
                blk[0] += 1
                load_w_block(c, wg[i], wgB[i], w_gu, j * 128, 128)
                load_w_block(c, wu[i], wuB[i], w_gu, F + j * 128, 128)
                return i
            nxt = wl(0)
            for j in range(nfc):
                cur = nxt
                if j + 1 < nfc:
                    nxt = wl(j + 1)
                bg, bu = (j % 2) * 2, (j % 2) * 2 + 1
                for kc in range(NKC):
                    p.op("pe", lambda e, kc=kc, cur=cur, bg=bg: e.matmul(c.PB[bg][:, :], lhsT=wg[cur][:, kc, :], rhs=hT[:, kc, :], start=(kc == 0), stop=(kc == NKC - 1)),
                         reads=[wgB[cur], hTB], writes=[c.PBb[bg]])
                for kc in range(NKC):
                    p.op("pe", lambda e, kc=kc, cur=cur, bu=bu: e.matmul(c.PB[bu][:, :], lhsT=wu[cur][:, kc, :], rhs=hT[:, kc, :], start=(kc == 0), stop=(kc == NKC - 1)),
                         reads=[wuB[cur], hTB], writes=[c.PBb[bu]])
                si = j % 2
                p.op("act", lambda e, bg=bg, si=si: e.activation(out=sg[si], in_=c.PB[bg][:, :], func=AF.Silu), reads=[c.PBb[bg]], writes=[sgB[si]])
                p.op("dve", lambda e, bu=bu, si=si, j=j: e.tensor_tensor(out=actT[:, j, :], in0=sg[si], in1=c.PB[bu][:, :], op=ALU.mult),
                     reads=[sgB[si], c.PBb[bu]], writes=[actB[j]])
            def dl(i_, hf):
                f0, f1 = (0, nh0) if hf == 0 else (nh0, nfc)
                src = w_dn[f0 * 128:f1 * 128, i_ * 128:(i_ + 1) * 128].rearrange("(fc p) c -> p fc c", p=128)
                p.dma("pool", lambda e, hf=hf, src=src: e.dma_start(out=wd[hf], in_=src), writes=[wdB[hf]])
            for i_ in range(NKC):
                bk = 4 + i_ % 2
                for hf in range(2):
                    dl(i_, hf)
                    f0, f1 = (0, nh0) if hf == 0 else (nh0, nfc)
                    for fc in range(f0, f1):
                        p.op("pe", lambda e, fc=fc, f0=f0, hf=hf, bk=bk: e.matmul(c.PB[bk][:, :], lhsT=wd[hf][:, fc - f0, :], rhs=actT[:, fc, :], start=(fc == 0), stop=(fc == nfc - 1)),
                             reads=[wdB[hf], actB[fc]], writes=[c.PBb[bk]])
                if (not moe) or (MOEDBG & 16):
                    evac(c, big32[:, i_, :], c.PB[bk][:, :], [c.PBb[bk]], [bigB[i_]])
                elif ex == 0:
                    p.op("dve", lambda e, i_=i_, bk=bk: e.tensor_tensor(out=big32[:, i_, :], in0=c.PB[bk][:, :], in1=gb, op=ALU.mult),
                         reads=[c.PBb[bk], gbB], writes=[bigB[i_]])
                else:
                    ti = i_ % 2
                    p.op("dve", lambda e, bk=bk, ti=ti: e.tensor_tensor(out=tmp[ti], in0=c.PB[bk][:, :], in1=gb, op=ALU.mult),
                         reads=[c.PBb[bk], gbB], writes=[tmpB[ti]])
                    p.op("pool", lambda e, i_=i_, ti=ti: e.tensor_tensor(out=big32[:, i_, :], in0=big32[:, i_, :], in1=tmp[ti], op=ALU.add),
                         reads=[tmpB[ti], bigB[i_]], writes=[bigB[i_]])
        for t in range(4):
            r0 = g * G + t * 128
            i = t % 2
            p.dma("sp", lambda e, r0=r0, i=i: e.dma_start(out=hin[i], in_=h_d[r0:r0 + 128, :]), writes=[hinB[i]])
            for i_ in range(NKC):
                bk = i_ // 4
                p.op("pe", lambda e, i_=i_, bk=bk, t=t: e.transpose(out=c.PB[bk][:, (i_ % 4) * 128:(i_ % 4 + 1) * 128], in_=big32[:, i_, t * 128:(t + 1) * 128], identity=c.id32),
                     reads=[bigB[i_], c.id_b], writes=[c.PBb[bk]])
            ln_epilogue(c, [0, 1, 2, 3], hin[i], hinB[i], out_d[r0:r0 + 128, :], gbc, bbc, lnB, lw[0])


KVDBG = 1


def kv_phase(c, h_d, w_kv, KT_d, V_d, km_d):
    p = c.p
    p.reset()
    hin = [p.sb(f"hin{i}", [128, D], F32) for i in range(2)]; hinB = [Buf(), Buf()]
    hT = p.sb("hT", [128, NKC, G], BF16); hTB = Buf()
    wkv = p.sb("wkv", [128, NKC, 1024], BF16); wkvB = [Buf(), Buf()]
    KTs = [p.sb(f"KTs{i}", [128, G], BF16) for i in range(2)]; KTsB = [Buf(), Buf()]
    Vs = [p.sb(f"Vs{i}", [128, 512], BF16) for i in range(2)]; VsB = [Buf(), Buf()]
    kms = p.sb("kms", [128, 4, 8], F32); kmB = Buf()
    outB = Buf()
    for s_ in range(2):
        load_w_block(c, wkv[:, :, s_ * 512:(s_ + 1) * 512], wkvB[s_], w_kv, s_ * 512, 512)
    n = 0
    for g in range(T // G):
        for t in range(4):
            i = t % 2
            r0 = g * G + t * 128
            p.dma("sp", lambda e, r0=r0, i=i: e.dma_start(out=hin[i], in_=h_d[r0:r0 + 128, :]), writes=[hinB[i]])
            transpose_tile32(c, hin[i], hinB[i], hT, hTB, t * 128, (4, 5, 6, 7))
        for hd in range(4):
            bk = hd % 2
            for kc in range(NKC):
                p.op("pe", lambda e, hd=hd, kc=kc, bk=bk: e.matmul(c.PB[bk][:, :], lhsT=wkv[:, kc, hd * 128:(hd + 1) * 128], rhs=hT[:, kc, :],
                                                                   start=(kc == 0), stop=(kc == NKC - 1)), reads=[wkvB[0], hTB], writes=[c.PBb[bk]])
            i = n % 2; n += 1
            for hf in range(2):
                p.op("act", lambda e, hd=hd, g=g, bk=bk, hf=hf, i=i: e.activation(
                    out=KTs[i][:, hf * 256:(hf + 1) * 256], in_=c.PB[bk][:, hf * 256:(hf + 1) * 256], func=AF.Copy,
                    accum_out=kms[:, hd, 2 * g + hf:2 * g + hf + 1]), reads=[c.PBb[bk]], writes=[KTsB[i], kmB])
            p.dma("sp", lambda e, hd=hd, g=g, i=i: e.dma_start(out=KT_d[:, hd, g * G:(g + 1) * G], in_=KTs[i]), reads=[KTsB[i]], writes=[outB])
        for t in range(4):
            bk = 2 + t % 2
            for kc in range(NKC):
                p.op("pe", lambda e, t=t, kc=kc, bk=bk: e.matmul(c.PB[bk][:, :], lhsT=hT[:, kc, t * 128:(t + 1) * 128], rhs=wkv[:, kc, 512:1024],
                                                                 start=(kc == 0), stop=(kc == NKC - 1)), reads=[wkvB[1], hTB], writes=[c.PBb[bk]])
            i = t % 2
            evac(c, Vs[i], c.PB[bk][:, :], [c.PBb[bk]], [VsB[i]], eng="dve")
            p.dma("sp", lambda e, t=t, g=g, i=i: e.dma_start(out=V_d[g * 4 + t], in_=Vs[i]), reads=[VsB[i]], writes=[outB])
    p.op("dve", lambda e: e.tensor_scalar(out=kms.rearrange("p a b -> p (a b)"), in0=kms.rearrange("p a b -> p (a b)"), scalar1=1.0 / 256.0, scalar2=None, op0=ALU.mult),
         reads=[kmB], writes=[kmB])
    p.dma("sp", lambda e: e.dma_start(out=km_d, in_=kms), reads=[kmB], writes=[outB])


def bc_last(ap, n):
    d = [list(x) for x in ap.ap]
    return bass.AP(ap.tensor, ap.offset, d + [[0, n]])


def bc_mid(ap, n):
    d = [list(x) for x in ap.ap]
    return bass.AP(ap.tensor, ap.offset, [d[0], [0, n]] + d[1:])


def moba_phase(c, h_d, w_q, KT_all, V_all, km_all, gbias_d, cmask_d, aT_d):
    p = c.p
    p.reset()
    KT = p.sb("KT", [128, 4, 2 * T], BF16); KTB = Buf()
    Va = p.sb("Va", [128, 32, 4, 129], BF16); VaB = Buf()
    kmb = p.sb("kmb", [128, 4, 16], BF16); kmbB = Buf()
    gbs = p.sb("gbs", [128, 8, 16], F32); gbsB = Buf()
    cmask = p.sb("cmask", [128, 4, 128], F32); cmB = Buf()
    hin = [p.sb(f"hin{i}", [128, D], F32) for i in range(2)]; hinB = [Buf(), Buf()]
    hT = p.sb("hT", [128, NKC, G], BF16); hTB = Buf()
    wqb = [p.sb(f"wqb{i}", [128, NKC, 256], BF16) for i in range(2)]; wqbB = [Buf(), Buf()]
    qT = p.sb("qT", [128, 16, G], BF16); qTB = [Buf() for _ in range(16)]
    gm = p.sb("gm", [128, 16, 16], F32); mx8 = p.sb("mx8", [128, 16, 8], F32)
    sel = p.sb("sel", [128, 16, 16], F32); vld = p.sb("vld", [128, 16, 16], F32); selB = Buf()
    acc = p.sb("acc", [128, 16, 129], F32); accB = [Buf() for _ in range(16)]
    PT = [[p.sb(f"PT{i}{j}", [128, 4, 128], BF16) for j in range(2)] for i in range(2)]
    PTB = [[Buf(), Buf()], [Buf(), Buf()]]
    rec = p.sb("rec", [128, 16], F32); recB = Buf()
    attn = p.sb("attn", [128, D], BF16); attnB = Buf()
    aT = p.sb("aT", [128, NKC, 128], BF16); aTB = Buf()
    outB = Buf()
    for hd in range(4):
        p.dma("sp", lambda e, hd=hd: e.dma_start(out=KT[:, hd, :], in_=KT_all[:, hd, :]), writes=[KTB])
    p.op("pool", lambda e: e.memset(Va.rearrange("p a b c -> p (a b c)"), 1.0), writes=[VaB])
    for kt in range(32):
        p.dma("sp", lambda e, kt=kt: e.dma_start(out=Va[:, kt, :, 0:128], in_=V_all[kt].rearrange("p (h d) -> p h d", h=4)), writes=[VaB])
    p.dma("pool", lambda e: e.dma_start(out=kmb, in_=km_all), writes=[kmbB])
    p.dma("sp", lambda e: e.dma_start(out=gbs.rearrange("p a b -> p (a b)"), in_=bcast_rows(gbias_d, 128)), writes=[gbsB])
    for h in range(4):
        p.dma("sp", lambda e, h=h: e.dma_start(out=cmask[:, h, :], in_=cmask_d), writes=[cmB])
    wi = [0]
    QS = 128 ** -0.5
    blkn = [0]
    for g in range(T // G):
        for t in range(4):
            i = t % 2
            r0 = g * G + t * 128
            p.dma("sp", lambda e, r0=r0, i=i: e.dma_start(out=hin[i], in_=h_d[r0:r0 + 128, :]), writes=[hinB[i]])
            transpose_tile32(c, hin[i], hinB[i], hT, hTB, t * 128, (4, 5, 6, 7))
        def wl(b):
            i = wi[0] % 2
            wi[0] += 1
            load_w_block(c, wqb[i], wqbB[i], w_q, b * 256, 256)
            return i
        nxt = wl(0)
        for b in range(8):
            cur = nxt
            if b + 1 < 8:
                nxt = wl(b + 1)
            for j in range(2):
                h = 2 * b + j
                bk = h % 4
                for kc in range(NKC):
                    p.op("pe", lambda e, cur=cur, j=j, kc=kc, bk=bk: e.matmul(c.PB[bk][:, :], lhsT=wqb[cur][:, kc, j * 128:(j + 1) * 128], rhs=hT[:, kc, :],
                                                                            start=(kc == 0), stop=(kc == NKC - 1)), reads=[wqbB[cur], hTB], writes=[c.PBb[bk]])
                p.op("act", lambda e, h=h, bk=bk: e.activation(out=qT[:, h, :], in_=c.PB[bk][:, :], func=AF.Copy, scale=QS), reads=[c.PBb[bk]], writes=[qTB[h]])
        for t in range(4):
            ti = g * 4 + t
            qb = ti // 2
            tw = ti % 2
            ts_ = slice(t * 128, (t + 1) * 128)
            for h in range(16):
                p.op("pe", lambda e, h=h, ts_=ts_: e.matmul(c.PB[0][:, h * 16:(h + 1) * 16], lhsT=qT[:, h, ts_], rhs=kmb[:, h // 4, :], start=True, stop=True),
                     reads=[qTB[h], kmbB], writes=[c.PBb[0]])
            p.op("dve", lambda e, qb=qb: e.tensor_tensor(out=gm, in0=c.PB[0][:, 0:256].rearrange("p (a b) -> p a b", a=16), in1=bc_mid(gbs[:, qb, :], 16), op=ALU.add),
                 reads=[c.PBb[0], gbsB], writes=[selB])
            for h in range(16):
                p.op("dve", lambda e, h=h: e.max(out=mx8[:, h, :], in_=gm[:, h, :]), reads=[selB], writes=[selB])
            p.op("dve", lambda e: e.tensor_tensor(out=sel, in0=gm, in1=bc_last(mx8[:, :, 2], 16), op=ALU.is_ge), reads=[selB], writes=[selB])
            p.op("dve", lambda e: e.tensor_scalar(out=vld.rearrange("p a b -> p (a b)"), in0=gm.rearrange("p a b -> p (a b)"), scalar1=-1e29, scalar2=None, op0=ALU.is_gt),
                 reads=[selB], writes=[selB])
            p.op("dve", lambda e: e.tensor_tensor(out=sel, in0=sel, in1=vld, op=ALU.mult), reads=[selB], writes=[selB])
            slots = list(range(8)) + [8 + n_ for n_ in range(qb + 1)]
            for gh in range(4):
                for si, s_ in enumerate(slots):
                    diag = (s_ == 8 + qb)
                    kb = s_ * 256 if s_ < 8 else T + (s_ - 8) * 256
                    chunks = [0, 1]
                    if diag and tw == 0:
                        chunks = [0]
                    bi = blkn[0] % 2
                    blkn[0] += 1
                    for ck in chunks:
                        bk = 2 * bi + ck
                        k0 = kb + ck * 128
                        p.op("pe", lambda e, gh=gh, k0=k0, bk=bk, ts_=ts_: e.matmul(c.PB[bk][:, :].rearrange("p (a b) -> p a b", a=4), lhsT=KT[:, gh, k0:k0 + 128], rhs=qT[:, 4 * gh:4 * gh + 4, ts_], start=True, stop=True),
                             reads=[KTB] + qTB[4 * gh:4 * gh + 4], writes=[c.PBb[bk]])
                        pt = PT[bi][ck]
                        p.op("act", lambda e, bk=bk, pt=pt: e.activation(out=pt.rearrange("p a b -> p (a b)"), in_=c.PB[bk][:, :], func=AF.Exp), reads=[c.PBb[bk]], writes=[PTB[bi][ck]])
                        if diag and ck == tw:
                            p.op("dve", lambda e, pt=pt: e.tensor_tensor(out=pt, in0=pt, in1=cmask, op=ALU.mult), reads=[PTB[bi][ck], cmB], writes=[PTB[bi][ck]])
                    for hh in range(4):
                        bk = 4 + 2 * bi + hh // 2
                        cs_ = (hh % 2) * 129
                        for ci, ck in enumerate(chunks):
                            kt = (kb + ck * 128) // 128
                            p.op("pe", lambda e, bk=bk, cs_=cs_, hh=hh, ck=ck, kt=kt, gh=gh, bi=bi, ci=ci, nch=len(chunks): e.matmul(
                                c.PB[bk][:, cs_:cs_ + 129], lhsT=PT[bi][ck][:, hh, :], rhs=Va[:, kt, gh, :], start=(ci == 0), stop=(ci == nch - 1)),
                                reads=[PTB[bi][ck], VaB], writes=[c.PBb[bk]])
                    for hh in range(4):
                        h = 4 * gh + hh
                        bk = 4 + 2 * bi + hh // 2
                        cs_ = (hh % 2) * 129
                        if diag:
                            p.op("dve", lambda e, h=h, bk=bk, cs_=cs_: e.tensor_tensor(out=acc[:, h, :], in0=acc[:, h, :], in1=c.PB[bk][:, cs_:cs_ + 129], op=ALU.add),
                                 reads=[accB[h], c.PBb[bk]], writes=[accB[h]])
                        elif si == 0:
                            p.op("dve", lambda e, h=h, bk=bk, cs_=cs_, s_=s_: e.tensor_scalar(out=acc[:, h, :], in0=c.PB[bk][:, cs_:cs_ + 129], scalar1=sel[:, h, s_:s_ + 1], scalar2=None, op0=ALU.mult),
                                 reads=[selB, c.PBb[bk]], writes=[accB[h]])
                        else:
                            p.op("dve", lambda e, h=h, bk=bk, cs_=cs_, s_=s_: e.scalar_tensor_tensor(out=acc[:, h, :], in0=c.PB[bk][:, cs_:cs_ + 129], scalar=sel[:, h, s_:s_ + 1], in1=acc[:, h, :], op0=ALU.mult, op1=ALU.add),
                                 reads=[selB, c.PBb[bk], accB[h]], writes=[accB[h]])
            p.op("dve", lambda e: e.reciprocal(out=rec, in_=acc[:, :, 128]), reads=accB, writes=[recB])
            p.op("dve", lambda e: e.tensor_tensor(out=attn.rearrange("p (a b) -> p a b", a=16), in0=acc[:, :, 0:128], in1=bc_last(rec, 128), op=ALU.mult),
                 reads=accB + [recB], writes=[attnB])
            for half in range(2):
                bk = 2 + half
                pbv = c.PB[bk].bitcast(BF16)
                for j in range(8):
                    kc = half * 8 + j
                    p.op("pe", lambda e, pbv=pbv, j=j, kc=kc: e.transpose(out=pbv[:, j * 128:(j + 1) * 128], in_=attn[:, kc * 128:(kc + 1) * 128], identity=c.idb),
                         reads=[attnB, c.id_b], writes=[c.PBb[bk]])
                evac(c, aT[:, half * 8:half * 8 + 8, :], pbv[:, 0:1024].rearrange("p (a b) -> p a b", a=8), [c.PBb[bk]], [aTB], eng="act")
            p.dma("sp", lambda e, ti=ti: e.dma_start(out=aT_d[ti], in_=aT), reads=[aTB], writes=[outB])


def dram_in(nc, name, shape, dt=F32):
    return nc.dram_tensor(name, list(shape), dt, kind="ExternalInput").ap()


def dram_out(nc, name, shape, dt=F32):
    return nc.dram_tensor(name, list(shape), dt, kind="ExternalOutput").ap()


def build(mode="L1"):
    nc = bass.Bass("TRN2", target_bir_lowering=False)
    cst = {"ident": dram_in(nc, "ident", [128, 128]), "cmask": dram_in(nc, "cmask", [128, 128])}
    with contextlib.ExitStack() as st:
        p = Prog(nc, st)
        p.init_arena()
        c = setup_ctx(nc, p, cst)
        p.mark()
        ln_mix_g = dram_in(nc, "ln_mix_g", [2, D]); ln_mix_b = dram_in(nc, "ln_mix_b", [2, D])
        ln_ffn_g = dram_in(nc, "ln_ffn_g", [2, D]); ln_ffn_b = dram_in(nc, "ln_ffn_b", [2, D])
        aT_d = nc.dram_tensor("aT_d", [T // 128, 128, NKC, 128], BF16, kind="Internal").ap()
        if mode.startswith("T_"):
            x_own = dram_in(nc, "x_own", [T, D])
            if mode.startswith("T_kv"):
                w_kv = dram_in(nc, "w_kv", [D, 1024])
                KT_d = dram_out(nc, "KT_d", [128, 4, T], BF16); V_d = dram_out(nc, "V_d", [T // 128, 128, 512], BF16)
                km_d = dram_out(nc, "km_d", [128, 4, 8])
                kv_phase(c, x_own, w_kv, KT_d, V_d, km_d)
            if mode == "T_moe":
                esel = dram_in(nc, "esel", [8, NEXP, 128]); w_router = dram_in(nc, "w_router", [D, NEXP])
                w_gu_moe = dram_in(nc, "w_gu_moe", [1, D, 2 * F_EXP]); w_down_moe = dram_in(nc, "w_down_moe", [1, F_EXP, D])
                out = dram_out(nc, "out", [T, D])
                ffn_phase(c, x_own, [w_gu_moe[0]], [w_down_moe[0]], F_EXP, ln_ffn_g[1:2, :], ln_ffn_b[1:2, :], out, w_router=w_router, esel_d=esel)
            if mode == "T_ffn":
                w_gu_dense = dram_in(nc, "w_gu_dense", [D, 2 * F_DENSE]); w_down_dense = dram_in(nc, "w_down_dense", [F_DENSE, D])
                h1 = dram_out(nc, "h1", [T, D])
                ffn_phase(c, x_own, [w_gu_dense], [w_down_dense], F_DENSE, ln_ffn_g[0:1, :], ln_ffn_b[0:1, :], h1)
        elif mode in ("L1", "L1a", "L1b", "L1c"):
            x_prev = dram_in(nc, "x_prev", [T, D]); x_own = dram_in(nc, "x_own", [T, D])
            w_in = dram_in(nc, "w_in", [D, 6160]); w_gate_up = dram_in(nc, "w_gate_up", [16, 1024])
            b_gate = dram_in(nc, "b_gate", [1, 1024]); g_norm = dram_in(nc, "g_norm", [1, 512])
            w_o_a = dram_in(nc, "w_o_a", [D, D])
            w_gu_dense = dram_in(nc, "w_gu_dense", [D, 2 * F_DENSE]); w_down_dense = dram_in(nc, "w_down_dense", [F_DENSE, D])
            w_kv = dram_in(nc, "w_kv", [D, 1024])
            hm0 = dram_out(nc, "hm0", [T, D]); h1 = dram_out(nc, "h1", [T, D])
            KT_d = dram_out(nc, "KT_d", [128, 4, T], BF16); V_d = dram_out(nc, "V_d", [T // 128, 128, 512], BF16)
            km_d = dram_out(nc, "km_d", [128, 4, 8])
            gla_phase(c, x_prev, x_own, w_in, w_gate_up, b_gate, g_norm, cst["cmask"], aT_d)
            proj_ln_phase(c, aT_d, w_o_a, x_own, ln_mix_g[0:1, :], ln_mix_b[0:1, :], hm0)
            if mode in ("L1", "L1b"):
                ffn_phase(c, hm0, [w_gu_dense], [w_down_dense], F_DENSE, ln_ffn_g[0:1, :], ln_ffn_b[0:1, :], h1)
            if mode in ("L1", "L1c"):
                kv_phase(c, h1 if mode == "L1" else hm0, w_kv, KT_d, V_d, km_d)
        else:
            h1 = dram_in(nc, "h1", [T, D])
            KT_all = dram_in(nc, "KT_all", [128, 4, 2 * T], BF16); V_all = dram_in(nc, "V_all", [32, 128, 512], BF16)
            km_all = dram_in(nc, "km_all", [128, 4, 16]); gbias = dram_in(nc, "gbias", [1, 128])
            esel = dram_in(nc, "esel", [8, NEXP, 128])
            w_q = dram_in(nc, "w_q", [D, D]); w_o_b = dram_in(nc, "w_o_b", [D, D])
            hm1 = dram_out(nc, "hm1", [T, D]); out = dram_out(nc, "out", [T, D])
            if mode == "L2":
                w_router = dram_in(nc, "w_router", [D, NEXP])
                w_gu_moe = dram_in(nc, "w_gu_moe", [NEXP, D, 2 * F_EXP]); w_down_moe = dram_in(nc, "w_down_moe", [NEXP, F_EXP, D])
            moba_phase(c, h1, w_q, KT_all, V_all, km_all, gbias, cst["cmask"], aT_d)
            proj_ln_phase(c, aT_d, w_o_b, h1, ln_mix_g[1:2, :], ln_mix_b[1:2, :], hm1)
            if mode == "L2":
                ffn_phase(c, hm1, [w_gu_moe[e] for e in range(NEXP)], [w_down_moe[e] for e in range(NEXP)], F_EXP,
                          ln_ffn_g[1:2, :], ln_ffn_b[1:2, :], out, w_router=w_router, esel_d=esel)
        p.emit()
    return nc


_NC_CACHE = {}


def _get_nc(mode):
    if mode not in _NC_CACHE:
        _NC_CACHE[mode] = build(mode)
    return _NC_CACHE[mode]


def kernel(x, w_in_a, w_gate_up_a, b_gate_a, g_norm_a, w_o_a, w_kv_shared, w_q_b, w_o_b,
           ln_mix_g, ln_mix_b, w_gu_dense, w_down_dense, w_router, w_gu_moe, w_down_moe,
           ln_ffn_g, ln_ffn_b):
    f = lambda a: np.ascontiguousarray(np.asarray(a, dtype=np.float32))
    x = f(x)
    ident = np.eye(128, dtype=np.float32)
    cmask = np.triu(np.ones((128, 128), np.float32))
    common = {"ident": ident, "cmask": cmask, "ln_mix_g": f(ln_mix_g), "ln_mix_b": f(ln_mix_b),
              "ln_ffn_g": f(ln_ffn_g), "ln_ffn_b": f(ln_ffn_b)}
    w1 = {"w_in": f(w_in_a[0]), "w_gate_up": f(w_gate_up_a[0]), "b_gate": f(b_gate_a).reshape(1, 1024),
          "g_norm": f(g_norm_a).reshape(1, 512), "w_o_a": f(w_o_a[0]), "w_gu_dense": f(w_gu_dense[0]),
          "w_down_dense": f(w_down_dense[0]), "w_kv": f(w_kv_shared)}
    ins1 = []
    for core in range(8):
        b, half = core // 2, core % 2
        xo = x[b, half * T:(half + 1) * T]
        xp = x[b, 0:T] if half == 1 else np.zeros_like(xo)
        d = {"x_prev": np.ascontiguousarray(xp), "x_own": np.ascontiguousarray(xo)}
        d.update(common); d.update(w1)
        ins1.append(d)
    r1 = run_bass_kernel_spmd(_get_nc("L1"), ins1, core_ids=list(range(8))).results
    esel = np.zeros((8, NEXP, 128), np.float32)
    for e in range(NEXP):
        esel[e, e, :] = 1.0
    w2 = {"w_q": f(w_q_b[0]), "w_o_b": f(w_o_b[0]), "w_router": f(w_router[0]), "w_gu_moe": f(w_gu_moe[0]),
          "w_down_moe": f(w_down_moe[0]), "esel": esel}
    ins2 = []
    for core in range(8):
        half = core % 2
        prev = core - 1 if half == 1 else core
        gb = np.full((8, 16), -1e30, np.float32)
        for qb in range(8):
            if half == 1:
                gb[qb, 0:8] = 0.0
            gb[qb, 8:8 + qb] = 0.0
        d = {"h1": r1[core]["h1"],
             "KT_all": np.ascontiguousarray(np.concatenate([r1[prev]["KT_d"], r1[core]["KT_d"]], axis=2)),
             "V_all": np.ascontiguousarray(np.concatenate([r1[prev]["V_d"], r1[core]["V_d"]], axis=0)),
             "km_all": np.ascontiguousarray(np.concatenate([r1[prev]["km_d"], r1[core]["km_d"]], axis=2)),
             "gbias": gb.reshape(1, 128)}
        d.update(common); d.update(w2)
        ins2.append(d)
    r2 = run_bass_kernel_spmd(_get_nc("L2"), ins2, core_ids=list(range(8))).results
    out = np.empty((4, 2 * T, D), np.float32)
    for core in range(8):
        b, half = core // 2, core % 2
        out[b, half * T:(half + 1) * T] = r2[core]["out"]
    return out
```

```python
import contextlib
import math
import numpy as np
import concourse.bass as bass
import concourse.mybir as mybir
from concourse.bass_utils import run_bass_kernel_spmd

F32 = mybir.dt.float32
BF16 = mybir.dt.bfloat16
AF = mybir.ActivationFunctionType
ALU = mybir.AluOpType
AX = mybir.AxisListType

ENGS = ("pe", "act", "dve", "pool", "sp")

D = 2048
T = 2048
NKC = 16
G = 512
ALPHA = (2 * 2) ** 0.25
LN_EPS = 1e-5
RMS_EPS = 1e-6
F_DENSE = 5504
F_EXP = 7168
NEXP = 8


class Buf:
    __slots__ = ("name", "w", "r")

    def __init__(self, name=""):
        self.name = name
        self.w = None
        self.r = {}


class Prog:
    def __init__(self, nc, stack, n_dma_sems=16):
        self.nc = nc
        self.stack = stack
        self.q = {e: [] for e in ENGS}
        self.sems = {}
        self.cnt = {}
        for e in ENGS:
            self.sems["E" + e] = stack.enter_context(nc.semaphore("prog_" + e))
            self.cnt["E" + e] = 0
        self.dma_ring = {}
        for e in ("sp", "pool", "act"):
            keys = []
            for i in range(n_dma_sems):
                k = f"D{e}{i}"
                self.sems[k] = stack.enter_context(nc.semaphore(f"dma_{e}{i}"))
                self.cnt[k] = 0
                keys.append(k)
            self.dma_ring[e] = [keys, 0]
        self.nops = 0
        self.floor = {}
        self.floor_pending = set()

    def init_arena(self, nwords=44032):
        self.big = self.stack.enter_context(self.nc.sbuf_tensor("arena", [128, nwords], F32))
        self.a_words = nwords
        self.a_off = 0
        self.a_mark = 0

    def sb(self, name, shape, dt):
        esz = 2 if dt == BF16 else 4
        n = 1
        for s_ in shape[1:]:
            n *= s_
        words = (n * esz + 3) // 4
        words = (words + 7) // 8 * 8
        assert self.a_off + words <= self.a_words, f"SBUF arena overflow at {name}: {self.a_off}+{words}"
        v = self.big[0:shape[0], self.a_off:self.a_off + words]
        self.a_off += words
        if dt != F32:
            v = v.bitcast(dt)
        v = v[:, 0:n]
        if len(shape) == 3:
            v = v.rearrange("p (a b) -> p a b", a=shape[1])
        elif len(shape) == 4:
            v = v.rearrange("p (a b c) -> p a b c", a=shape[1], b=shape[2])
        return v

    def mark(self):
        self.a_mark = self.a_off

    def reset(self):
        self.a_off = self.a_mark
        self.floor = dict(self.cnt)
        self.floor_pending = set(ENGS)

    def ps(self, name, shape, dt=F32):
        return self.stack.enter_context(self.nc.psum_tensor(name, list(shape), dt))

    def _collect(self, eng, reads, writes, is_dma):
        waits = {}
        if eng in self.floor_pending:
            self.floor_pending.discard(eng)
            waits.update({k: v for k, v in self.floor.items() if v > 0})

        def need(sem, val, src, war=False):
            if src == eng and not is_dma:
                if eng == "pe" or war:
                    return
            if val > waits.get(sem, 0):
                waits[sem] = val

        for b in reads:
            if b.w is not None:
                need(*b.w)
        for b in writes:
            if b.w is not None:
                need(*b.w)
            for sem, (val, src) in b.r.items():
                need(sem, val, src, war=True)
        return waits

    def _commit(self, reads, writes, sem, val, eng):
        for b in reads:
            old = b.r.get(sem)
            if old is None or old[0] < val:
                b.r[sem] = (val, eng)
        for b in writes:
            b.w = (sem, val, eng)
            b.r = {}

    def op(self, eng, fn, reads=(), writes=()):
        waits = self._collect(eng, reads, writes, False)
        sem = "E" + eng
        self.cnt[sem] += 1
        val = self.cnt[sem]
        self.q[eng].append((waits, fn, sem, 1))
        self._commit(reads, writes, sem, val, eng)
        self.nops += 1

    def dma(self, eng, fn, reads=(), writes=()):
        waits = self._collect(eng, reads, writes, True)
        ring = self.dma_ring[eng]
        sem = ring[0][ring[1] % len(ring[0])]
        ring[1] += 1
        prev = self.cnt[sem]
        if prev > waits.get(sem, 0):
            waits[sem] = prev
        self.cnt[sem] += 16
        val = self.cnt[sem]
        self.q[eng].append((waits, fn, sem, 16))
        self._commit(reads, writes, sem, val, eng)
        self.nops += 1

    def emit(self):
        nc = self.nc
        engobj = {"pe": "tensor", "act": "scalar", "dve": "vector", "pool": "gpsimd", "sp": "sync"}
        final = dict(self.cnt)
        with nc.Block() as block:
            for e in ENGS:
                items = self.q[e]

                def body(eo, items=items, e=e):
                    waited = {}
                    for waits, fn, sem, amt in items:
                        for s, v in waits.items():
                            if waited.get(s, 0) < v:
                                eo.wait_ge(self.sems[s], v)
                                waited[s] = v
                        ins = fn(eo)
                        ins.then_inc(self.sems[sem], amt)
                    if e == "sp":
                        for s, v in final.items():
                            if v > 0 and waited.get(s, 0) < v:
                                eo.wait_ge(self.sems[s], v)

                getattr(block, engobj[e])(body)


def bcast_rows(ap_row, n):
    return bass.AP(ap_row.tensor, ap_row.offset, [[0, 128], [1, n]])


class Ctx:
    pass


def setup_ctx(nc, p, cst):
    c = Ctx()
    c.nc, c.p = nc, p
    c.PB = [p.ps(f"pb{i}", [128, 512]) for i in range(8)]
    c.PBb = [Buf(f"pb{i}") for i in range(8)]
    c.id32 = p.sb("id32", [128, 128], F32)
    c.idb = p.sb("idb", [128, 128], BF16)
    c.id_b = Buf("ident")
    p.dma("sp", lambda e: e.dma_start(out=c.id32[:], in_=cst["ident"]), writes=[c.id_b])
    p.dma("pool", lambda e: e.dma_start(out=c.idb[:], in_=cst["ident"]), writes=[c.id_b])
    c.rr = 0
    return c


def evac(c, out, in_, reads, writes, eng=None):
    p = c.p
    if eng is None:
        eng = ("act", "dve")[c.rr % 2]
        c.rr += 1
    if eng == "act":
        p.op("act", lambda e: e.activation(out=out, in_=in_, func=AF.Copy), reads=reads, writes=writes)
    else:
        p.op("dve", lambda e: e.tensor_copy(out=out, in_=in_), reads=reads, writes=writes)


def transpose_tile32(c, src, srcB, dstb, dstB, col0, banks, dst32=None, dst32B=None):
    p = c.p
    for b in range(4):
        bk = banks[b]
        for j in range(4):
            kc = 4 * b + j
            p.op("pe", lambda e, bk=bk, j=j, kc=kc: e.transpose(
                out=c.PB[bk][:, j * 128:(j + 1) * 128], in_=src[:, kc * 128:(kc + 1) * 128], identity=c.id32[:]),
                reads=[srcB, c.id_b], writes=[c.PBb[bk]])
        pv = c.PB[bk][:, :].rearrange("p (a b) -> p a b", a=4)
        evac(c, dstb[:, 4 * b:4 * b + 4, col0:col0 + 128], pv, [c.PBb[bk]], [dstB], eng="act")
        if dst32 is not None:
            evac(c, dst32[:, 4 * b:4 * b + 4, col0:col0 + 128], pv, [c.PBb[bk]], dst32B[4 * b:4 * b + 4], eng="act")


def load_w_block(c, dst, dstB, w, col0, ncols, nk=NKC):
    src = w[:, col0:col0 + ncols].rearrange("(kc p) c -> p kc c", p=128)
    c.p.dma("pool", lambda e: e.dma_start(out=dst, in_=src), writes=[dstB])


def ln_epilogue(c, banks, resid, residB, out_dram, gbc, bbc, lnB, W):
    p = c.p
    st, mv, rs, B2 = W["st"], W["mv"], W["rs"], W["B2"]
    t, tB = resid, residB
    for s in range(4):
        sl = slice(s * 512, (s + 1) * 512)
        p.op("dve", lambda e, s=s, sl=sl: e.scalar_tensor_tensor(
            out=t[:, sl], in0=t[:, sl], scalar=ALPHA, in1=c.PB[banks[s]][:, :], op0=ALU.mult, op1=ALU.add),
            reads=[tB, c.PBb[banks[s]]], writes=[tB])
    for s in range(4):
        p.op("dve", lambda e, s=s: e.bn_stats(out=st[:, s * 6:(s + 1) * 6], in_=t[:, s * 512:(s + 1) * 512]),
             reads=[tB], writes=[B2])
    p.op("dve", lambda e: e.bn_aggr(out=mv, in_=st), reads=[B2], writes=[B2])
    p.op("act", lambda e: e.activation(out=rs, in_=mv[:, 1:2], func=AF.Sqrt, bias=LN_EPS, scale=1.0),
         reads=[B2], writes=[B2])
    p.op("dve", lambda e: e.reciprocal(out=rs, in_=rs), reads=[B2], writes=[B2])
    p.op("dve", lambda e: e.tensor_scalar(out=t, in0=t, scalar1=mv[:, 0:1], scalar2=rs[:, 0:1],
                                          op0=ALU.subtract, op1=ALU.mult), reads=[tB, B2], writes=[tB])
    p.op("pool", lambda e: e.tensor_tensor(out=t, in0=t, in1=gbc, op=ALU.mult), reads=[tB, lnB], writes=[tB])
    p.op("pool", lambda e: e.tensor_tensor(out=t, in0=t, in1=bbc, op=ALU.add), reads=[tB, lnB], writes=[tB])
    p.dma("sp", lambda e: e.dma_start(out=out_dram, in_=t), reads=[tB], writes=[W["outB"]])


def ln_work(p, tag, outB):
    return {"st": p.sb(f"lnst{tag}", [128, 24], F32),
            "mv": p.sb(f"lnmv{tag}", [128, 2], F32), "rs": p.sb(f"lnrs{tag}", [128, 1], F32), "B2": Buf(),
            "outB": outB}


def load_ln(c, g_row, b_row, gbc, bbc, lnB):
    c.p.dma("sp", lambda e: e.dma_start(out=gbc, in_=bcast_rows(g_row, D)), writes=[lnB])
    c.p.dma("sp", lambda e: e.dma_start(out=bbc, in_=bcast_rows(b_row, D)), writes=[lnB])


def gla_phase(c, x_cat, w_in, w_gate_up, b_gate, g_norm, cmask_d, oT_d, first_out=4):
    p = c.p
    p.reset()
    xin = p.sb("xin", [128, D], F32); xinB = Buf()
    xT = p.sb("xT", [128, NKC, G], BF16); xTB = Buf()
    wblk = [p.sb(f"wblk{i}", [128, NKC, 256], BF16) for i in range(2)]; wblkB = [Buf(), Buf()]
    wz = p.sb("wz", [128, NKC, 16], BF16); wzB = Buf()
    wgu = p.sb("wgu", [17, 1024], BF16); wguB = Buf()
    zT = p.sb("zT", [17, G], BF16); zTB = Buf()
    cs = p.sb("cs", [128, 8, G], F32); csB = [Buf() for _ in range(8)]
    dec = p.sb("dec", [128, 8, 4], F32)
    e1 = [p.sb(f"e1_{i}", [128, G], F32) for i in range(2)]; e1B = [Buf(), Buf()]
    ones = p.sb("ones", [128, 128], F32); onesB = Buf()
    qdT = p.sb("qdT", [128, 8, G], BF16); qdB = [Buf() for _ in range(8)]
    kiT = p.sb("kiT", [128, 8, G], BF16); kiB = [Buf() for _ in range(8)]
    keT = p.sb("keT", [128, 8, G], BF16); keB = [Buf() for _ in range(8)]
    v = p.sb("v", [128, 4, D], BF16); vB = [Buf() for _ in range(4)]
    sr = p.sb("sr", [128, 4, D], BF16); srB = [Buf() for _ in range(4)]
    gnbc = p.sb("gnbc", [128, D], F32); gnB = Buf()
    stmp = [p.sb(f"stmp{i}", [128, 256], F32) for i in range(2)]; stmpB = [Buf(), Buf()]
    S = p.sb("S", [128, 8, 512], F32); SB = [Buf() for _ in range(8)]
    Sb = p.sb("Sb", [128, 8, 512], BF16); SbB = [Buf() for _ in range(8)]
    ke = p.sb("ke", [128, 1024], BF16); keTB = Buf()
    attT = p.sb("attT", [128, 4, 128], BF16); attB = Buf()
    cmask = p.sb("cmask", [128, 4, 128], F32); cmB = Buf()
    junk = p.sb("junk", [128, 512], F32); junkB = Buf()
    ss = p.sb("ss", [128, 4], F32); rst = p.sb("rst", [128, 4], F32); ssB = Buf()
    ofin = p.sb("ofin", [128, D], BF16); ofinB = Buf()
    ofT = p.sb("ofT", [128, NKC, 128], BF16); ofTB = Buf()
    outB = Buf()

    p.dma("pool", lambda e: e.dma_start(out=wz, in_=w_in[:, 6144:6160].rearrange("(kc p) c -> p kc c", p=128)), writes=[wzB])
    p.dma("pool", lambda e: e.dma_start(out=wgu[0:16, :], in_=w_gate_up), writes=[wguB])
    p.dma("pool", lambda e: e.dma_start(out=wgu[16:17, :], in_=b_gate), writes=[wguB])
    for h in range(4):
        p.dma("sp", lambda e, h=h: e.dma_start(out=gnbc[:, h * 512:(h + 1) * 512], in_=bcast_rows(g_norm, 512)), writes=[gnB])
    for h in range(4):
        p.dma("sp", lambda e, h=h: e.dma_start(out=cmask[:, h, :], in_=cmask_d), writes=[cmB])
    p.op("pool", lambda e: e.memset(ones, 1.0), writes=[onesB])
    p.op("pool", lambda e: e.memset(zT, 1.0), writes=[zTB])
    p.op("pool", lambda e: e.memset(S.rearrange("p a b -> p (a b)"), 0.0), writes=SB)
    p.op("pool", lambda e: e.memset(Sb.rearrange("p a b -> p (a b)"), 0.0), writes=SbB)

    LNSC = math.log(256 ** -0.5)
    wi = [0]

    def wload(col0):
        i = wi[0] % 2
        wi[0] += 1
        load_w_block(c, wblk[i], wblkB[i], w_in, col0, 256)
        return i

    for g in range(8):
        own = g >= first_out
        xsrc = x_cat
        g4 = g
        for t in range(4):
            r0 = g4 * G + t * 128
            p.dma("sp", lambda e, r0=r0, xsrc=xsrc: e.dma_start(out=xin, in_=xsrc[r0:r0 + 128, :]), writes=[xinB])
            transpose_tile32(c, xin, xinB, xT, xTB, t * 128, (4, 5, 6, 7))
        for kc in range(NKC):
            p.op("pe", lambda e, kc=kc: e.matmul(c.PB[0][0:16, :], lhsT=wz[:, kc, :], rhs=xT[:, kc, :], start=(kc == 0), stop=(kc == NKC - 1)),
                 reads=[wzB, xTB], writes=[c.PBb[0]])
        evac(c, zT[0:16, :], c.PB[0][0:16, :], [c.PBb[0]], [zTB], eng="act")
        for dk in range(8):
            bk = 1 + dk % 2
            p.op("pe", lambda e, dk=dk, bk=bk: e.matmul(c.PB[bk][:, :], lhsT=wgu[:, dk * 128:(dk + 1) * 128], rhs=zT, start=True, stop=True),
                 reads=[wguB, zTB], writes=[c.PBb[bk]])
            a, b = e1[0], e1[1]
            p.op("act", lambda e, bk=bk, a=a: e.activation(out=a, in_=c.PB[bk][:, :], func=AF.Exp, scale=-1.0), reads=[c.PBb[bk]], writes=[e1B[0]])
            p.op("act", lambda e, a=a, b=b: e.activation(out=b, in_=a, func=AF.Ln, bias=1.0, scale=1.0), reads=[e1B[0]], writes=[e1B[1]])
            for ch in range(4):
                p.op("dve", lambda e, dk=dk, ch=ch, b=b: e.tensor_tensor_scan(
                    out=cs[:, dk, ch * 128:(ch + 1) * 128], data0=ones, data1=b[:, ch * 128:(ch + 1) * 128], initial=0.0,
                    op0=ALU.mult, op1=ALU.add), reads=[onesB, e1B[1]], writes=[csB[dk]])
            p.op("act", lambda e, dk=dk: e.activation(
                out=dec[:, dk, :], in_=cs[:, dk, :].rearrange("p (a b) -> p a b", a=4)[:, :, 127], func=AF.Exp, scale=-1.0 / 16.0),
                reads=[csB[dk]], writes=[csB[dk]])
        kinds = [("k", b) for b in range(4)] + ([("q", b) for b in range(4)] if own else [])
        kinds += [("v", b) for b in range(8)] + ([("r", b) for b in range(8)] if own else [])
        base = {"q": 0, "k": 1024, "v": 2048, "r": 4096}
        nxt = wload(base[kinds[0][0]] + kinds[0][1] * 256)
        for bi, (kind, b) in enumerate(kinds):
            cur = nxt
            if bi + 1 < len(kinds):
                nxt = wload(base[kinds[bi + 1][0]] + kinds[bi + 1][1] * 256)
            W = wblk[cur]
            if kind in ("q", "k"):
                for j in range(2):
                    cc = 2 * b + j
                    bk = (2 * bi + j) % 4
                    for kc in range(NKC):
                        p.op("pe", lambda e, W=W, j=j, kc=kc, bk=bk: e.matmul(
                            c.PB[bk][:, :], lhsT=W[:, kc, j * 128:(j + 1) * 128], rhs=xT[:, kc, :], start=(kc == 0), stop=(kc == NKC - 1)),
                            reads=[wblkB[cur], xTB], writes=[c.PBb[bk]])
                    et = e1[0]
                    if kind == "q":
                        p.op("act", lambda e, cc=cc, et=et: e.activation(out=et, in_=cs[:, cc, :], func=AF.Exp, scale=-1.0 / 16.0, bias=LNSC),
                             reads=[csB[cc]], writes=[e1B[0]])
                        p.op("dve", lambda e, cc=cc, bk=bk, et=et: e.tensor_tensor(out=qdT[:, cc, :], in0=c.PB[bk][:, :], in1=et, op=ALU.mult),
                             reads=[c.PBb[bk], e1B[0]], writes=[qdB[cc]])
                    else:
                        p.op("act", lambda e, cc=cc, et=et: e.activation(out=et, in_=cs[:, cc, :], func=AF.Exp, scale=1.0 / 16.0),
                             reads=[csB[cc]], writes=[e1B[0]])
                        p.op("dve", lambda e, cc=cc, bk=bk, et=et: e.tensor_tensor(out=kiT[:, cc, :], in0=c.PB[bk][:, :], in1=et, op=ALU.mult),
                             reads=[c.PBb[bk], e1B[0]], writes=[kiB[cc]])
                        for ch in range(4):
                            sl = slice(ch * 128, (ch + 1) * 128)
                            p.op("dve", lambda e, cc=cc, bk=bk, et=et, ch=ch, sl=sl: e.scalar_tensor_tensor(
                                out=keT[:, cc, sl], in0=c.PB[bk][:, sl], scalar=dec[:, cc, ch:ch + 1], in1=et[:, sl], op0=ALU.mult, op1=ALU.mult),
                                reads=[c.PBb[bk], e1B[0], csB[cc]], writes=[keB[cc]])
            else:
                col0 = b * 256
                for t in range(4):
                    bk = 4 + (t // 2) + 2 * (bi % 2)
                    half = (t % 2) * 256
                    for kc in range(NKC):
                        p.op("pe", lambda e, W=W, t=t, kc=kc, bk=bk, half=half: e.matmul(
                            c.PB[bk][:, half:half + 256], lhsT=xT[:, kc, t * 128:(t + 1) * 128], rhs=W[:, kc, :], start=(kc == 0), stop=(kc == NKC - 1)),
                            reads=[wblkB[cur], xTB], writes=[c.PBb[bk]])
                    if kind == "v":
                        evac(c, v[:, t, col0:col0 + 256], c.PB[bk][:, half:half + 256], [c.PBb[bk]], [vB[t]], eng="act")
                    else:
                        si = t % 2
                        p.op("act", lambda e, bk=bk, half=half, si=si: e.activation(out=stmp[si], in_=c.PB[bk][:, half:half + 256], func=AF.Silu),
                             reads=[c.PBb[bk]], writes=[stmpB[si]])
                        p.op("pool", lambda e, t=t, col0=col0, si=si: e.tensor_tensor(out=sr[:, t, col0:col0 + 256], in0=stmp[si], in1=gnbc[:, col0:col0 + 256], op=ALU.mult),
                             reads=[stmpB[si], gnB], writes=[srB[t]])
        for ch in range(4):
            sl = slice(ch * 128, (ch + 1) * 128)
            if own:
                for h in range(4):
                    for tcl in range(2):
                        tc = 2 * h + tcl
                        p.op("pe", lambda e, h=h, tc=tc, tcl=tcl, sl=sl: e.matmul(
                            c.PB[4][:, h * 128:(h + 1) * 128], lhsT=kiT[:, tc, sl], rhs=qdT[:, tc, sl], start=(tcl == 0), stop=(tcl == 1)),
                            reads=[kiB[tc], qdB[tc]], writes=[c.PBb[4]])
                p.op("dve", lambda e: e.tensor_tensor(out=attT, in0=c.PB[4][:, :].rearrange("p (a b) -> p a b", a=4), in1=cmask, op=ALU.mult),
                     reads=[c.PBb[4], cmB], writes=[attB])
                for h in range(4):
                    p.op("pe", lambda e, h=h, ch=ch: e.matmul(c.PB[h][:, :], lhsT=attT[:, h, :], rhs=v[:, ch, h * 512:(h + 1) * 512], start=True, stop=False),
                         reads=[attB, vB[ch]], writes=[c.PBb[h]])
                    for tcl in range(2):
                        tc = 2 * h + tcl
                        p.op("pe", lambda e, h=h, tc=tc, tcl=tcl, sl=sl: e.matmul(c.PB[h][:, :], lhsT=qdT[:, tc, sl], rhs=Sb[:, tc, :], start=False, stop=(tcl == 1)),
                             reads=[qdB[tc], SbB[tc]], writes=[c.PBb[h]])
            pb7 = c.PB[7].bitcast(BF16)
            for tc in range(8):
                p.op("pe", lambda e, tc=tc, sl=sl: e.transpose(out=pb7[:, tc * 128:(tc + 1) * 128], in_=keT[:, tc, sl], identity=c.idb),
                     reads=[keB[tc], c.id_b], writes=[c.PBb[7]])
            evac(c, ke, pb7[:, 0:1024], [c.PBb[7]], [keTB], eng="act")
            for tc in range(8):
                h = tc // 2
                bk = 5 + tc % 2
                p.op("pe", lambda e, tc=tc, h=h, bk=bk, ch=ch: e.matmul(c.PB[bk][:, :], lhsT=ke[:, tc * 128:(tc + 1) * 128], rhs=v[:, ch, h * 512:(h + 1) * 512], start=True, stop=True),
                     reads=[keTB, vB[ch]], writes=[c.PBb[bk]])
                p.op("dve", lambda e, tc=tc, bk=bk, ch=ch: e.scalar_tensor_tensor(
                    out=S[:, tc, :], in0=S[:, tc, :], scalar=dec[:, tc, ch:ch + 1], in1=c.PB[bk][:, :], op0=ALU.mult, op1=ALU.add),
                    reads=[SB[tc], c.PBb[bk], csB[tc]], writes=[SB[tc]])
                p.op("pool", lambda e, tc=tc: e.tensor_copy(out=Sb[:, tc, :], in_=S[:, tc, :]), reads=[SB[tc]], writes=[SbB[tc]])
            if own:
                for h in range(4):
                    p.op("act", lambda e, h=h: e.activation(out=junk, in_=c.PB[h][:, :], func=AF.Square, accum_out=ss[:, h:h + 1]),
                         reads=[c.PBb[h]], writes=[junkB, ssB])
                p.op("act", lambda e: e.activation(out=rst, in_=ss, func=AF.Sqrt, scale=1.0 / 512.0, bias=RMS_EPS), reads=[ssB], writes=[ssB])
                p.op("dve", lambda e: e.reciprocal(out=rst, in_=rst), reads=[ssB], writes=[ssB])
                for h in range(4):
                    hs = slice(h * 512, (h + 1) * 512)
                    p.op("dve", lambda e, h=h, hs=hs, ch=ch: e.scalar_tensor_tensor(
                        out=ofin[:, hs], in0=c.PB[h][:, :], scalar=rst[:, h:h + 1], in1=sr[:, ch, hs], op0=ALU.mult, op1=ALU.mult),
                        reads=[c.PBb[h], ssB, srB[ch]], writes=[ofinB])
                for half in range(2):
                    bk = 5 + half
                    pbv = c.PB[bk].bitcast(BF16)
                    for j in range(8):
                        kc = half * 8 + j
                        p.op("pe", lambda e, pbv=pbv, j=j, kc=kc: e.transpose(out=pbv[:, j * 128:(j + 1) * 128], in_=ofin[:, kc * 128:(kc + 1) * 128], identity=c.idb),
                             reads=[ofinB, c.id_b], writes=[c.PBb[bk]])
                    evac(c, ofT[:, half * 8:half * 8 + 8, :], pbv[:, 0:1024].rearrange("p (a b) -> p a b", a=8), [c.PBb[bk]], [ofTB], eng="act")
                tile = (g - first_out) * 4 + ch
                p.dma("sp", lambda e, tile=tile: e.dma_start(out=oT_d[tile], in_=ofT), reads=[ofTB], writes=[outB])


def proj_ln_phase(c, aT_d, w, resid_d, g_row, b_row, out_d, ntiles=T // 128):
    p = c.p
    p.reset()
    W = p.sb("W", [128, NKC, D], BF16); WB = [Buf() for _ in range(4)]
    aT = [p.sb(f"aT{i}", [128, NKC, 128], BF16) for i in range(2)]; aTB = [Buf(), Buf()]
    res = [p.sb(f"res{i}", [128, D], F32) for i in range(2)]; resB = [Buf(), Buf()]
    gbc = p.sb("gbc", [128, D], F32); bbc = p.sb("bbc", [128, D], F32); lnB = Buf()
    outB = Buf()
    lw = [ln_work(p, i, outB) for i in range(2)]
    for s in range(4):
        load_w_block(c, W[:, :, s * 512:(s + 1) * 512], WB[s], w, s * 512, 512)
    load_ln(c, g_row, b_row, gbc, bbc, lnB)
    for t in range(ntiles):
        i = t % 2
        p.dma("sp", lambda e, t=t, i=i: e.dma_start(out=aT[i], in_=aT_d[t]), writes=[aTB[i]])
        p.dma("sp", lambda e, t=t, i=i: e.dma_start(out=res[i], in_=resid_d[t * 128:(t + 1) * 128, :]), writes=[resB[i]])
        banks = [4 * i + s for s in range(4)]
        for s in range(4):
            for kc in range(NKC):
                p.op("pe", lambda e, s=s, kc=kc, i=i, bk=banks[s]: e.matmul(
                    c.PB[bk][:, :], lhsT=aT[i][:, kc, :], rhs=W[:, kc, s * 512:(s + 1) * 512], start=(kc == 0), stop=(kc == NKC - 1)),
                    reads=[aTB[i], WB[s]], writes=[c.PBb[banks[s]]])
        ln_epilogue(c, banks, res[i], resB[i], out_d[t * 128:(t + 1) * 128, :], gbc, bbc, lnB, lw[i])


MOEDBG = 0


def ffn_phase(c, h_d, w_gu_list, w_d_list, F, g_row, b_row, out_d, w_router=None, esel_d=None, ntok=T):
    p = c.p
    p.reset()
    moe = w_router is not None
    nfc = F // 128
    hin = [p.sb("hin0", [128, D], F32)] * 2; hinB = [Buf()] * 2
    hT = p.sb("hT", [128, NKC, G], BF16); hTB = Buf()
    big32 = p.sb("big32", [128, NKC, G], F32); bigB = [Buf() for _ in range(NKC)]
    wg = [p.sb(f"wg{i}", [128, NKC, 128], BF16) for i in range(2)]; wgB = [Buf(), Buf()]
    wu = [p.sb(f"wu{i}", [128, NKC, 128], BF16) for i in range(2)]; wuB = [Buf(), Buf()]
    nh0 = (nfc + 1) // 2
    wd = [p.sb("wd0", [128, nh0, 128], BF16), p.sb("wd1", [128, nfc - nh0, 128], BF16)]; wdB = [Buf(), Buf()]
    actT = p.sb("actT", [128, nfc, G], BF16); actB = [Buf() for _ in range(nfc)]
    sg = [p.sb("sg0", [128, G], F32)] * 2; sgB = [Buf()] * 2
    gbc = p.sb("gbc", [128, D], F32); bbc = p.sb("bbc", [128, D], F32); lnB = Buf()
    outB = Buf()
    lw = [ln_work(p, 0, outB)]
    load_ln(c, g_row, b_row, gbc, bbc, lnB)
    if moe:
        wr = p.sb("wr", [128, NKC, NEXP], F32); wrB = Buf()
        esel = p.sb("esel", [8, NEXP, 128], F32); eselB = Buf()
        p.dma("sp", lambda e: e.dma_start(out=wr, in_=w_router.rearrange("(kc p) c -> p kc c", p=128)), writes=[wrB])
        p.dma("sp", lambda e: e.dma_start(out=esel, in_=esel_d), writes=[eselB])
        lg = p.sb("lg", [128, 8], F32); mx = p.sb("mx", [128, 8], F32); el = p.sb("el", [128, 8], F32)
        msk = p.sb("msk", [128, 8], F32); sm = p.sb("sm", [128, 4], F32); gts = p.sb("gts", [128, 8], F32); rB = Buf()
        gT = p.sb("gT", [8, G], F32); gTB = Buf()
        gb = p.sb("gb", [128, G], F32); gbB = Buf()
        tmp = [p.sb("tmp0", [128, G], F32)] * 2; tmpB = [Buf()] * 2
    n_exp = len(w_gu_list)
    for g in range(ntok // G):
        for t in range(4):
            i = t % 2
            r0 = g * G + t * 128
            p.dma("sp", lambda e, r0=r0, i=i: e.dma_start(out=hin[i], in_=h_d[r0:r0 + 128, :]), writes=[hinB[i]])
            transpose_tile32(c, hin[i], hinB[i], hT, hTB, t * 128, (4, 5, 6, 7), dst32=big32 if (moe and not (MOEDBG & 8)) else None,
                             dst32B=bigB)
        if moe:
            for t in range(4):
                if MOEDBG & 1:
                    p.op("dve", lambda e: e.tensor_copy(out=lg, in_=wr[:, 0, :]), reads=[wrB], writes=[rB])
                else:
                    for kc in range(NKC):
                        p.op("pe", lambda e, t=t, kc=kc: e.matmul(c.PB[0][:, 0:8], lhsT=big32[:, kc, t * 128:(t + 1) * 128], rhs=wr[:, kc, :],
                                                                  start=(kc == 0), stop=(kc == NKC - 1)),
                             reads=[bigB[kc], wrB], writes=[c.PBb[0]])
                    p.op("dve", lambda e: e.tensor_copy(out=lg, in_=c.PB[0][:, 0:8]), reads=[c.PBb[0]], writes=[rB])
                p.op("dve", lambda e: e.max(out=mx, in_=lg), reads=[rB], writes=[rB])
                p.op("dve", lambda e: e.tensor_scalar(out=sm[:, 0:1], in0=mx[:, 0:1], scalar1=-1.0, scalar2=None, op0=ALU.mult), reads=[rB], writes=[rB])
                p.op("act", lambda e: e.activation(out=el, in_=lg, func=AF.Exp, bias=sm[:, 0:1], scale=1.0), reads=[rB], writes=[rB])
                p.op("act", lambda e: e.activation(out=sm[:, 1:2], in_=mx[:, 1:2], func=AF.Exp, bias=sm[:, 0:1], scale=1.0), reads=[rB], writes=[rB])
                p.op("dve", lambda e: e.tensor_scalar(out=sm[:, 2:3], in0=sm[:, 1:2], scalar1=1.0, scalar2=None, op0=ALU.add), reads=[rB], writes=[rB])
                p.op("dve", lambda e: e.reciprocal(out=sm[:, 3:4], in_=sm[:, 2:3]), reads=[rB], writes=[rB])
                p.op("dve", lambda e: e.tensor_scalar(out=msk, in0=lg, scalar1=mx[:, 1:2], scalar2=None, op0=ALU.is_ge), reads=[rB], writes=[rB])
                p.op("dve", lambda e: e.scalar_tensor_tensor(out=gts, in0=el, scalar=sm[:, 3:4], in1=msk, op0=ALU.mult, op1=ALU.mult), reads=[rB], writes=[rB])
                if not (MOEDBG & 2):
                    p.op("pe", lambda e, t=t: e.transpose(out=c.PB[1][0:8, t * 128:(t + 1) * 128], in_=gts, identity=c.id32), reads=[rB, c.id_b], writes=[c.PBb[1]])
            if MOEDBG & 2:
                p.op("dve", lambda e: e.memset(gT, 0.5), reads=[rB], writes=[gTB])
            else:
                evac(c, gT, c.PB[1][0:8, :], [c.PBb[1]], [gTB], eng="dve")
        blk = [0]
        for ex in range(n_exp):
            w_gu, w_dn = w_gu_list[ex], w_d_list[ex]
            if moe and (MOEDBG & 4):
                p.op("dve", lambda e: e.memset(gb, 0.5), reads=[gTB], writes=[gbB])
            elif moe:
                p.op("pe", lambda e, ex=ex: e.matmul(c.PB[0][:, :], lhsT=esel[:, ex, :], rhs=gT, start=True, stop=True), reads=[eselB, gTB], writes=[c.PBb[0]])
                evac(c, gb, c.PB[0][:, :], [c.PBb[0]], [gbB], eng="act")
            def wl(j):
                i = blk[0] % 2
                blk[0] += 1
                load_w_block(c, wg[i], wgB[i], w_gu, j * 128, 128)
                load_w_block(c, wu[i], wuB[i], w_gu, F + j * 128, 128)
                return i
            nxt = wl(0)
            for j in range(nfc):
                cur = nxt
                if j + 1 < nfc:
                    nxt = wl(j + 1)
                bg, bu = (j % 2) * 2, (j % 2) * 2 + 1
                for kc in range(NKC):
                    p.op("pe", lambda e, kc=kc, cur=cur, bg=bg: e.matmul(c.PB[bg][:, :], lhsT=wg[cur][:, kc, :], rhs=hT[:, kc, :], start=(kc == 0), stop=(kc == NKC - 1)),
                         reads=[wgB[cur], hTB], writes=[c.PBb[bg]])
                for kc in range(NKC):
                    p.op("pe", lambda e, kc=kc, cur=cur, bu=bu: e.matmul(c.PB[bu][:, :], lhsT=wu[cur][:, kc, :], rhs=hT[:, kc, :], start=(kc == 0), stop=(kc == NKC - 1)),
                         reads=[wuB[cur], hTB], writes=[c.PBb[bu]])
                si = j % 2
                p.op("act", lambda e, bg=bg, si=si: e.activation(out=sg[si], in_=c.PB[bg][:, :], func=AF.Silu), reads=[c.PBb[bg]], writes=[sgB[si]])
                p.op("dve", lambda e, bu=bu, si=si, j=j: e.tensor_tensor(out=actT[:, j, :], in0=sg[si], in1=c.PB[bu][:, :], op=ALU.mult),
                     reads=[sgB[si], c.PBb[bu]], writes=[actB[j]])
            def dl(i_, hf):
                f0, f1 = (0, nh0) if hf == 0 else (nh0, nfc)
                src = w_dn[f0 * 128:f1 * 128, i_ * 128:(i_ + 1) * 128].rearrange("(fc p) c -> p fc c", p=128)
                p.dma("pool", lambda e, hf=hf, src=src: e.dma_start(out=wd[hf], in_=src), writes=[wdB[hf]])
            for i_ in range(NKC):
                bk = 4 + i_ % 2
                for hf in range(2):
                    dl(i_, hf)
                    f0, f1 = (0, nh0) if hf == 0 else (nh0, nfc)
                    for fc in range(f0, f1):
                        p.op("pe", lambda e, fc=fc, f0=f0, hf=hf, bk=bk: e.matmul(c.PB[bk][:, :], lhsT=wd[hf][:, fc - f0, :], rhs=actT[:, fc, :], start=(fc == 0), stop=(fc == nfc - 1)),
                             reads=[wdB[hf], actB[fc]], writes=[c.PBb[bk]])
                if (not moe) or (MOEDBG & 16):
                    evac(c, big32[:, i_, :], c.PB[bk][:, :], [c.PBb[bk]], [bigB[i_]])
                elif ex == 0:
                    p.op("dve", lambda e, i_=i_, bk=bk: e.tensor_tensor(out=big32[:, i_, :], in0=c.PB[bk][:, :], in1=gb, op=ALU.mult),
                         reads=[c.PBb[bk], gbB], writes=[bigB[i_]])
                else:
                    ti = i_ % 2
                    p.op("dve", lambda e, bk=bk, ti=ti: e.tensor_tensor(out=tmp[ti], in0=c.PB[bk][:, :], in1=gb, op=ALU.mult),
                         reads=[c.PBb[bk], gbB], writes=[tmpB[ti]])
                    p.op("pool", lambda e, i_=i_, ti=ti: e.tensor_tensor(out=big32[:, i_, :], in0=big32[:, i_, :], in1=tmp[ti], op=ALU.add),
                         reads=[tmpB[ti], bigB[i_]], writes=[bigB[i_]])
        for t in range(4):
            r0 = g * G + t * 128
            i = t % 2
            p.dma("sp", lambda e, r0=r0, i=i: e.dma_start(out=hin[i], in_=h_d[r0:r0 + 128, :]), writes=[hinB[i]])
            for i_ in range(NKC):
                bk = i_ // 4
                p.op("pe", lambda e, i_=i_, bk=bk, t=t: e.transpose(out=c.PB[bk][:, (i_ % 4) * 128:(i_ % 4 + 1) * 128], in_=big32[:, i_, t * 128:(t + 1) * 128], identity=c.id32),
                     reads=[bigB[i_], c.id_b], writes=[c.PBb[bk]])
            ln_epilogue(c, [0, 1, 2, 3], hin[i], hinB[i], out_d[r0:r0 + 128, :], gbc, bbc, lnB, lw[0])


KVDBG = 1


def kv_phase(c, h_d, w_kv, KT_d, V_d, km_d, ntok=T):
    p = c.p
    p.reset()
    hin = [p.sb(f"hin{i}", [128, D], F32) for i in range(2)]; hinB = [Buf(), Buf()]
    hT = p.sb("hT", [128, NKC, G], BF16); hTB = Buf()
    wkv = p.sb("wkv", [128, NKC, 1024], BF16); wkvB = [Buf(), Buf()]
    KTs = [p.sb(f"KTs{i}", [128, G], BF16) for i in range(2)]; KTsB = [Buf(), Buf()]
    Vs = [p.sb(f"Vs{i}", [128, 512], BF16) for i in range(2)]; VsB = [Buf(), Buf()]
    kms = p.sb("kms", [128, 4, ntok // 256], F32); kmB = Buf()
    outB = Buf()
    for s_ in range(2):
        load_w_block(c, wkv[:, :, s_ * 512:(s_ + 1) * 512], wkvB[s_], w_kv, s_ * 512, 512)
    n = 0
    for g in range(ntok // G):
        for t in range(4):
            i = t % 2
            r0 = g * G + t * 128
            p.dma("sp", lambda e, r0=r0, i=i: e.dma_start(out=hin[i], in_=h_d[r0:r0 + 128, :]), writes=[hinB[i]])
            transpose_tile32(c, hin[i], hinB[i], hT, hTB, t * 128, (4, 5, 6, 7))
        for hd in range(4):
            bk = hd % 2
            for kc in range(NKC):
                p.op("pe", lambda e, hd=hd, kc=kc, bk=bk: e.matmul(c.PB[bk][:, :], lhsT=wkv[:, kc, hd * 128:(hd + 1) * 128], rhs=hT[:, kc, :],
                                                                   start=(kc == 0), stop=(kc == NKC - 1)), reads=[wkvB[0], hTB], writes=[c.PBb[bk]])
            i = n % 2; n += 1
            for hf in range(2):
                p.op("act", lambda e, hd=hd, g=g, bk=bk, hf=hf, i=i: e.activation(
                    out=KTs[i][:, hf * 256:(hf + 1) * 256], in_=c.PB[bk][:, hf * 256:(hf + 1) * 256], func=AF.Copy,
                    accum_out=kms[:, hd, 2 * g + hf:2 * g + hf + 1]), reads=[c.PBb[bk]], writes=[KTsB[i], kmB])
            p.dma("sp", lambda e, hd=hd, g=g, i=i: e.dma_start(out=KT_d[:, hd, g * G:(g + 1) * G], in_=KTs[i]), reads=[KTsB[i]], writes=[outB])
        for t in range(4):
            bk = 2 + t % 2
            for kc in range(NKC):
                p.op("pe", lambda e, t=t, kc=kc, bk=bk: e.matmul(c.PB[bk][:, :], lhsT=hT[:, kc, t * 128:(t + 1) * 128], rhs=wkv[:, kc, 512:1024],
                                                                 start=(kc == 0), stop=(kc == NKC - 1)), reads=[wkvB[1], hTB], writes=[c.PBb[bk]])
            i = t % 2
            evac(c, Vs[i], c.PB[bk][:, :], [c.PBb[bk]], [VsB[i]], eng="dve")
            p.dma("sp", lambda e, t=t, g=g, i=i: e.dma_start(out=V_d[g * 4 + t], in_=Vs[i]), reads=[VsB[i]], writes=[outB])
    p.op("dve", lambda e: e.tensor_scalar(out=kms.rearrange("p a b -> p (a b)"), in0=kms.rearrange("p a b -> p (a b)"), scalar1=1.0 / 256.0, scalar2=None, op0=ALU.mult),
         reads=[kmB], writes=[kmB])
    p.dma("sp", lambda e: e.dma_start(out=km_d, in_=kms), reads=[kmB], writes=[outB])


def bc_last(ap, n):
    d = [list(x) for x in ap.ap]
    return bass.AP(ap.tensor, ap.offset, d + [[0, n]])


def bc_mid(ap, n):
    d = [list(x) for x in ap.ap]
    return bass.AP(ap.tensor, ap.offset, [d[0], [0, n]] + d[1:])


def moba_phase(c, h_d, w_q, KT_all, V_all, km_all, gbias_d, cmask_d, aT_d):
    p = c.p
    p.reset()
    KT = p.sb("KT", [128, 4, 2 * T], BF16); KTB = Buf()
    Va = p.sb("Va", [128, 32, 4, 129], BF16); VaB = Buf()
    kmb = p.sb("kmb", [128, 4, 16], BF16); kmbB = Buf()
    gbs = p.sb("gbs", [128, 8, 16], F32); gbsB = Buf()
    cmask = p.sb("cmask", [128, 4, 128], F32); cmB = Buf()
    hin = [p.sb(f"hin{i}", [128, D], F32) for i in range(2)]; hinB = [Buf(), Buf()]
    hT = p.sb("hT", [128, NKC, G], BF16); hTB = Buf()
    wqb = [p.sb(f"wqb{i}", [128, NKC, 256], BF16) for i in range(2)]; wqbB = [Buf(), Buf()]
    qT = p.sb("qT", [128, 16, G], BF16); qTB = [Buf() for _ in range(16)]
    gm = p.sb("gm", [128, 16, 16], F32); mx8 = p.sb("mx8", [128, 16, 8], F32)
    sel = p.sb("sel", [128, 16, 16], F32); vld = p.sb("vld", [128, 16, 16], F32); selB = Buf()
    acc = p.sb("acc", [128, 16, 129], F32); accB = [Buf() for _ in range(16)]
    PT = [[p.sb(f"PT{i}{j}", [128, 4, 128], BF16) for j in range(2)] for i in range(2)]
    PTB = [[Buf(), Buf()], [Buf(), Buf()]]
    rec = p.sb("rec", [128, 16], F32); recB = Buf()
    attn = p.sb("attn", [128, D], BF16); attnB = Buf()
    aT = p.sb("aT", [128, NKC, 128], BF16); aTB = Buf()
    outB = Buf()
    for hd in range(4):
        p.dma("sp", lambda e, hd=hd: e.dma_start(out=KT[:, hd, :], in_=KT_all[:, hd, :]), writes=[KTB])
    p.op("pool", lambda e: e.memset(Va.rearrange("p a b c -> p (a b c)"), 1.0), writes=[VaB])
    for kt in range(32):
        p.dma("sp", lambda e, kt=kt: e.dma_start(out=Va[:, kt, :, 0:128], in_=V_all[kt].rearrange("p (h d) -> p h d", h=4)), writes=[VaB])
    p.dma("pool", lambda e: e.dma_start(out=kmb, in_=km_all), writes=[kmbB])
    p.dma("sp", lambda e: e.dma_start(out=gbs.rearrange("p a b -> p (a b)"), in_=bcast_rows(gbias_d, 128)), writes=[gbsB])
    for h in range(4):
        p.dma("sp", lambda e, h=h: e.dma_start(out=cmask[:, h, :], in_=cmask_d), writes=[cmB])
    wi = [0]
    QS = 128 ** -0.5
    blkn = [0]
    for g in range(T // G):
        for t in range(4):
            i = t % 2
            r0 = g * G + t * 128
            p.dma("sp", lambda e, r0=r0, i=i: e.dma_start(out=hin[i], in_=h_d[r0:r0 + 128, :]), writes=[hinB[i]])
            transpose_tile32(c, hin[i], hinB[i], hT, hTB, t * 128, (4, 5, 6, 7))
        def wl(b):
            i = wi[0] % 2
            wi[0] += 1
            load_w_block(c, wqb[i], wqbB[i], w_q, b * 256, 256)
            return i
        nxt = wl(0)
        for b in range(8):
            cur = nxt
            if b + 1 < 8:
                nxt = wl(b + 1)
            for j in range(2):
                h = 2 * b + j
                bk = h % 4
                for kc in range(NKC):
                    p.op("pe", lambda e, cur=cur, j=j, kc=kc, bk=bk: e.matmul(c.PB[bk][:, :], lhsT=wqb[cur][:, kc, j * 128:(j + 1) * 128], rhs=hT[:, kc, :],
                                                                            start=(kc == 0), stop=(kc == NKC - 1)), reads=[wqbB[cur], hTB], writes=[c.PBb[bk]])
                p.op("act", lambda e, h=h, bk=bk: e.activation(out=qT[:, h, :], in_=c.PB[bk][:, :], func=AF.Copy, scale=QS), reads=[c.PBb[bk]], writes=[qTB[h]])
        for t in range(4):
            ti = g * 4 + t
            qb = ti // 2
            tw = ti % 2
            ts_ = slice(t * 128, (t + 1) * 128)
            for h in range(16):
                p.op("pe", lambda e, h=h, ts_=ts_: e.matmul(c.PB[0][:, h * 16:(h + 1) * 16], lhsT=qT[:, h, ts_], rhs=kmb[:, h // 4, :], start=True, stop=True),
                     reads=[qTB[h], kmbB], writes=[c.PBb[0]])
            p.op("dve", lambda e, qb=qb: e.tensor_tensor(out=gm, in0=c.PB[0][:, 0:256].rearrange("p (a b) -> p a b", a=16), in1=bc_mid(gbs[:, qb, :], 16), op=ALU.add),
                 reads=[c.PBb[0], gbsB], writes=[selB])
            for h in range(16):
                p.op("dve", lambda e, h=h: e.max(out=mx8[:, h, :], in_=gm[:, h, :]), reads=[selB], writes=[selB])
            p.op("dve", lambda e: e.tensor_tensor(out=sel, in0=gm, in1=bc_last(mx8[:, :, 2], 16), op=ALU.is_ge), reads=[selB], writes=[selB])
            p.op("dve", lambda e: e.tensor_scalar(out=vld.rearrange("p a b -> p (a b)"), in0=gm.rearrange("p a b -> p (a b)"), scalar1=-1e29, scalar2=None, op0=ALU.is_gt),
                 reads=[selB], writes=[selB])
            p.op("dve", lambda e: e.tensor_tensor(out=sel, in0=sel, in1=vld, op=ALU.mult), reads=[selB], writes=[selB])
            slots = list(range(8)) + [8 + n_ for n_ in range(qb + 1)]
            for gh in range(4):
                for si, s_ in enumerate(slots):
                    diag = (s_ == 8 + qb)
                    kb = s_ * 256 if s_ < 8 else T + (s_ - 8) * 256
                    chunks = [0, 1]
                    if diag and tw == 0:
                        chunks = [0]
                    bi = blkn[0] % 2
                    blkn[0] += 1
                    for ck in chunks:
                        bk = 2 * bi + ck
                        k0 = kb + ck * 128
                        p.op("pe", lambda e, gh=gh, k0=k0, bk=bk, ts_=ts_: e.matmul(c.PB[bk][:, :].rearrange("p (a b) -> p a b", a=4), lhsT=KT[:, gh, k0:k0 + 128], rhs=qT[:, 4 * gh:4 * gh + 4, ts_], start=True, stop=True),
                             reads=[KTB] + qTB[4 * gh:4 * gh + 4], writes=[c.PBb[bk]])
                        pt = PT[bi][ck]
                        p.op("act", lambda e, bk=bk, pt=pt: e.activation(out=pt.rearrange("p a b -> p (a b)"), in_=c.PB[bk][:, :], func=AF.Exp), reads=[c.PBb[bk]], writes=[PTB[bi][ck]])
                        if diag and ck == tw:
                            p.op("dve", lambda e, pt=pt: e.tensor_tensor(out=pt, in0=pt, in1=cmask, op=ALU.mult), reads=[PTB[bi][ck], cmB], writes=[PTB[bi][ck]])
                    for hh in range(4):
                        bk = 4 + 2 * bi + hh // 2
                        cs_ = (hh % 2) * 129
                        for ci, ck in enumerate(chunks):
                            kt = (kb + ck * 128) // 128
                            p.op("pe", lambda e, bk=bk, cs_=cs_, hh=hh, ck=ck, kt=kt, gh=gh, bi=bi, ci=ci, nch=len(chunks): e.matmul(
                                c.PB[bk][:, cs_:cs_ + 129], lhsT=PT[bi][ck][:, hh, :], rhs=Va[:, kt, gh, :], start=(ci == 0), stop=(ci == nch - 1)),
                                reads=[PTB[bi][ck], VaB], writes=[c.PBb[bk]])
                    for hh in range(4):
                        h = 4 * gh + hh
                        bk = 4 + 2 * bi + hh // 2
                        cs_ = (hh % 2) * 129
                        if diag:
                            p.op("dve", lambda e, h=h, bk=bk, cs_=cs_: e.tensor_tensor(out=acc[:, h, :], in0=acc[:, h, :], in1=c.PB[bk][:, cs_:cs_ + 129], op=ALU.add),
                                 reads=[accB[h], c.PBb[bk]], writes=[accB[h]])
                        elif si == 0:
                            p.op("dve", lambda e, h=h, bk=bk, cs_=cs_, s_=s_: e.tensor_scalar(out=acc[:, h, :], in0=c.PB[bk][:, cs_:cs_ + 129], scalar1=sel[:, h, s_:s_ + 1], scalar2=None, op0=ALU.mult),
                                 reads=[selB, c.PBb[bk]], writes=[accB[h]])
                        else:
                            p.op("dve", lambda e, h=h, bk=bk, cs_=cs_, s_=s_: e.scalar_tensor_tensor(out=acc[:, h, :], in0=c.PB[bk][:, cs_:cs_ + 129], scalar=sel[:, h, s_:s_ + 1], in1=acc[:, h, :], op0=ALU.mult, op1=ALU.add),
                                 reads=[selB, c.PBb[bk], accB[h]], writes=[accB[h]])
            p.op("dve", lambda e: e.reciprocal(out=rec, in_=acc[:, :, 128]), reads=accB, writes=[recB])
            p.op("dve", lambda e: e.tensor_tensor(out=attn.rearrange("p (a b) -> p a b", a=16), in0=acc[:, :, 0:128], in1=bc_last(rec, 128), op=ALU.mult),
                 reads=accB + [recB], writes=[attnB])
            for half in range(2):
                bk = 2 + half
                pbv = c.PB[bk].bitcast(BF16)
                for j in range(8):
                    kc = half * 8 + j
                    p.op("pe", lambda e, pbv=pbv, j=j, kc=kc: e.transpose(out=pbv[:, j * 128:(j + 1) * 128], in_=attn[:, kc * 128:(kc + 1) * 128], identity=c.idb),
                         reads=[attnB, c.id_b], writes=[c.PBb[bk]])
                evac(c, aT[:, half * 8:half * 8 + 8, :], pbv[:, 0:1024].rearrange("p (a b) -> p a b", a=8), [c.PBb[bk]], [aTB], eng="act")
            p.dma("sp", lambda e, ti=ti: e.dma_start(out=aT_d[ti], in_=aT), reads=[aTB], writes=[outB])


def dram_in(nc, name, shape, dt=F32):
    return nc.dram_tensor(name, list(shape), dt, kind="ExternalInput").ap()


def dram_out(nc, name, shape, dt=F32):
    return nc.dram_tensor(name, list(shape), dt, kind="ExternalOutput").ap()


def build(mode="L1"):
    nc = bass.Bass("TRN2", target_bir_lowering=False)
    cst = {"ident": dram_in(nc, "ident", [128, 128]), "cmask": dram_in(nc, "cmask", [128, 128])}
    with contextlib.ExitStack() as st:
        p = Prog(nc, st)
        p.init_arena()
        c = setup_ctx(nc, p, cst)
        p.mark()
        ln_mix_g = dram_in(nc, "ln_mix_g", [2, D]); ln_mix_b = dram_in(nc, "ln_mix_b", [2, D])
        ln_ffn_g = dram_in(nc, "ln_ffn_g", [2, D]); ln_ffn_b = dram_in(nc, "ln_ffn_b", [2, D])
        aT_d = nc.dram_tensor("aT_d", [2 * T // 128, 128, NKC, 128], BF16, kind="Internal").ap()
        if mode == "F":
            x_cat = dram_in(nc, "x_cat", [2 * T, D])
            w_in = dram_in(nc, "w_in", [D, 6160]); w_gate_up = dram_in(nc, "w_gate_up", [16, 1024])
            b_gate = dram_in(nc, "b_gate", [1, 1024]); g_norm = dram_in(nc, "g_norm", [1, 512])
            w_o_a = dram_in(nc, "w_o_a", [D, D])
            w_gu_dense = dram_in(nc, "w_gu_dense", [D, 2 * F_DENSE]); w_down_dense = dram_in(nc, "w_down_dense", [F_DENSE, D])
            w_kv = dram_in(nc, "w_kv", [D, 1024])
            gbias = dram_in(nc, "gbias", [1, 128]); esel = dram_in(nc, "esel", [8, NEXP, 128])
            w_q = dram_in(nc, "w_q", [D, D]); w_o_b = dram_in(nc, "w_o_b", [D, D])
            w_router = dram_in(nc, "w_router", [D, NEXP])
            w_gu_moe = dram_in(nc, "w_gu_moe", [NEXP, D, 2 * F_EXP]); w_down_moe = dram_in(nc, "w_down_moe", [NEXP, F_EXP, D])
            out = dram_out(nc, "out", [T, D])
            hm0 = nc.dram_tensor("hm0", [2 * T, D], F32, kind="Internal").ap()
            h1 = nc.dram_tensor("h1", [2 * T, D], F32, kind="Internal").ap()
            hm1 = nc.dram_tensor("hm1", [T, D], F32, kind="Internal").ap()
            KT_all = nc.dram_tensor("KT_all", [128, 4, 2 * T], BF16, kind="Internal").ap()
            V_all = nc.dram_tensor("V_all", [32, 128, 512], BF16, kind="Internal").ap()
            km_all = nc.dram_tensor("km_all", [128, 4, 16], F32, kind="Internal").ap()
            gla_phase(c, x_cat, w_in, w_gate_up, b_gate, g_norm, cst["cmask"], aT_d, first_out=0)
            proj_ln_phase(c, aT_d, w_o_a, x_cat, ln_mix_g[0:1, :], ln_mix_b[0:1, :], hm0, ntiles=2 * T // 128)
            ffn_phase(c, hm0, [w_gu_dense], [w_down_dense], F_DENSE, ln_ffn_g[0:1, :], ln_ffn_b[0:1, :], h1, ntok=2 * T)
            kv_phase(c, h1, w_kv, KT_all, V_all, km_all, ntok=2 * T)
            h1o = h1[T:2 * T, :]
            moba_phase(c, h1o, w_q, KT_all, V_all, km_all, gbias, cst["cmask"], aT_d)
            proj_ln_phase(c, aT_d, w_o_b, h1o, ln_mix_g[1:2, :], ln_mix_b[1:2, :], hm1)
            ffn_phase(c, hm1, [w_gu_moe[e] for e in range(NEXP)], [w_down_moe[e] for e in range(NEXP)], F_EXP,
                      ln_ffn_g[1:2, :], ln_ffn_b[1:2, :], out, w_router=w_router, esel_d=esel)
        elif mode.startswith("T_"):
            x_own = dram_in(nc, "x_own", [T, D])
            if mode.startswith("T_kv"):
                w_kv = dram_in(nc, "w_kv", [D, 1024])
                KT_d = dram_out(nc, "KT_d", [128, 4, T], BF16); V_d = dram_out(nc, "V_d", [T // 128, 128, 512], BF16)
                km_d = dram_out(nc, "km_d", [128, 4, 8])
                kv_phase(c, x_own, w_kv, KT_d, V_d, km_d)
            if mode == "T_moe":
                esel = dram_in(nc, "esel", [8, NEXP, 128]); w_router = dram_in(nc, "w_router", [D, NEXP])
                w_gu_moe = dram_in(nc, "w_gu_moe", [1, D, 2 * F_EXP]); w_down_moe = dram_in(nc, "w_down_moe", [1, F_EXP, D])
                out = dram_out(nc, "out", [T, D])
                ffn_phase(c, x_own, [w_gu_moe[0]], [w_down_moe[0]], F_EXP, ln_ffn_g[1:2, :], ln_ffn_b[1:2, :], out, w_router=w_router, esel_d=esel)
            if mode == "T_ffn":
                w_gu_dense = dram_in(nc, "w_gu_dense", [D, 2 * F_DENSE]); w_down_dense = dram_in(nc, "w_down_dense", [F_DENSE, D])
                h1 = dram_out(nc, "h1", [T, D])
                ffn_phase(c, x_own, [w_gu_dense], [w_down_dense], F_DENSE, ln_ffn_g[0:1, :], ln_ffn_b[0:1, :], h1)
        elif mode in ("L1", "L1a", "L1b", "L1c"):
            x_cat = dram_in(nc, "x_cat", [2 * T, D]); x_own = x_cat[T:2 * T, :]
            w_in = dram_in(nc, "w_in", [D, 6160]); w_gate_up = dram_in(nc, "w_gate_up", [16, 1024])
            b_gate = dram_in(nc, "b_gate", [1, 1024]); g_norm = dram_in(nc, "g_norm", [1, 512])
            w_o_a = dram_in(nc, "w_o_a", [D, D])
            w_gu_dense = dram_in(nc, "w_gu_dense", [D, 2 * F_DENSE]); w_down_dense = dram_in(nc, "w_down_dense", [F_DENSE, D])
            w_kv = dram_in(nc, "w_kv", [D, 1024])
            hm0 = dram_out(nc, "hm0", [T, D]); h1 = dram_out(nc, "h1", [T, D])
            KT_d = dram_out(nc, "KT_d", [128, 4, T], BF16); V_d = dram_out(nc, "V_d", [T // 128, 128, 512], BF16)
            km_d = dram_out(nc, "km_d", [128, 4, 8])
            gla_phase(c, x_cat, w_in, w_gate_up, b_gate, g_norm, cst["cmask"], aT_d)
            proj_ln_phase(c, aT_d, w_o_a, x_own, ln_mix_g[0:1, :], ln_mix_b[0:1, :], hm0)
            if mode in ("L1", "L1b"):
                ffn_phase(c, hm0, [w_gu_dense], [w_down_dense], F_DENSE, ln_ffn_g[0:1, :], ln_ffn_b[0:1, :], h1)
            if mode in ("L1", "L1c"):
                kv_phase(c, h1 if mode == "L1" else hm0, w_kv, KT_d, V_d, km_d)
        else:
            h1 = dram_in(nc, "h1", [T, D])
            KT_all = dram_in(nc, "KT_all", [128, 4, 2 * T], BF16); V_all = dram_in(nc, "V_all", [32, 128, 512], BF16)
            km_all = dram_in(nc, "km_all", [128, 4, 16]); gbias = dram_in(nc, "gbias", [1, 128])
            esel = dram_in(nc, "esel", [8, NEXP, 128])
            w_q = dram_in(nc, "w_q", [D, D]); w_o_b = dram_in(nc, "w_o_b", [D, D])
            hm1 = dram_out(nc, "hm1", [T, D]); out = dram_out(nc, "out", [T, D])
            if mode == "L2":
                w_router = dram_in(nc, "w_router", [D, NEXP])
                w_gu_moe = dram_in(nc, "w_gu_moe", [NEXP, D, 2 * F_EXP]); w_down_moe = dram_in(nc, "w_down_moe", [NEXP, F_EXP, D])
            moba_phase(c, h1, w_q, KT_all, V_all, km_all, gbias, cst["cmask"], aT_d)
            proj_ln_phase(c, aT_d, w_o_b, h1, ln_mix_g[1:2, :], ln_mix_b[1:2, :], hm1)
            if mode == "L2":
                ffn_phase(c, hm1, [w_gu_moe[e] for e in range(NEXP)], [w_down_moe[e] for e in range(NEXP)], F_EXP,
                          ln_ffn_g[1:2, :], ln_ffn_b[1:2, :], out, w_router=w_router, esel_d=esel)
        p.emit()
    return nc


_NC_CACHE = {}


def _get_nc(mode):
    if mode not in _NC_CACHE:
        _NC_CACHE[mode] = build(mode)
    return _NC_CACHE[mode]


def kernel(x, w_in_a, w_gate_up_a, b_gate_a, g_norm_a, w_o_a, w_kv_shared, w_q_b, w_o_b,
           ln_mix_g, ln_mix_b, w_gu_dense, w_down_dense, w_router, w_gu_moe, w_down_moe,
           ln_ffn_g, ln_ffn_b):
    f = lambda a: np.ascontiguousarray(np.asarray(a, dtype=np.float32))
    x = f(x)
    esel = np.zeros((8, NEXP, 128), np.float32)
    for e in range(NEXP):
        esel[e, e, :] = 1.0
    shared = {"ident": np.eye(128, dtype=np.float32), "cmask": np.triu(np.ones((128, 128), np.float32)),
              "ln_mix_g": f(ln_mix_g), "ln_mix_b": f(ln_mix_b), "ln_ffn_g": f(ln_ffn_g), "ln_ffn_b": f(ln_ffn_b),
              "w_in": f(w_in_a[0]), "w_gate_up": f(w_gate_up_a[0]), "b_gate": f(b_gate_a).reshape(1, 1024),
              "g_norm": f(g_norm_a).reshape(1, 512), "w_o_a": f(w_o_a[0]), "w_gu_dense": f(w_gu_dense[0]),
              "w_down_dense": f(w_down_dense[0]), "w_kv": f(w_kv_shared),
              "w_q": f(w_q_b[0]), "w_o_b": f(w_o_b[0]), "w_router": f(w_router[0]), "w_gu_moe": f(w_gu_moe[0]),
              "w_down_moe": f(w_down_moe[0]), "esel": esel}
    ins = []
    for core in range(8):
        b, half = core // 2, core % 2
        xc = np.zeros((2 * T, D), np.float32)
        if half == 1:
            xc[0:T] = x[b, 0:T]
        xc[T:2 * T] = x[b, half * T:(half + 1) * T]
        gb = np.full((8, 16), -1e30, np.float32)
        for qb in range(8):
            if half == 1:
                gb[qb, 0:8] = 0.0
            gb[qb, 8:8 + qb] = 0.0
        d = {"x_cat": xc, "gbias": gb.reshape(1, 128)}
        d.update(shared)
        ins.append(d)
    res = run_bass_kernel_spmd(_get_nc("F"), ins, core_ids=list(range(8))).results
    out = np.empty((4, 2 * T, D), np.float32)
    for core in range(8):
        b, half = core // 2, core % 2
        out[b, half * T:(half + 1) * T] = res[core]["out"]
    return out
```

```python
import contextlib
import math
import numpy as np
import concourse.bass as bass
import concourse.mybir as mybir
from concourse.bass_utils import run_bass_kernel_spmd

F32 = mybir.dt.float32
BF16 = mybir.dt.bfloat16
AF = mybir.ActivationFunctionType
ALU = mybir.AluOpType
AX = mybir.AxisListType

ENGS = ("pe", "act", "dve", "pool", "sp")

D = 2048
T = 2048
NKC = 16
G = 512
ALPHA = (2 * 2) ** 0.25
LN_EPS = 1e-5
RMS_EPS = 1e-6
F_DENSE = 5504
F_EXP = 7168
NEXP = 8


class Buf:
    __slots__ = ("name", "w", "r")

    def __init__(self, name=""):
        self.name = name
        self.w = None
        self.r = {}


class Prog:
    def __init__(self, nc, stack, n_dma_sems=16):
        self.nc = nc
        self.stack = stack
        self.q = {e: [] for e in ENGS}
        self.sems = {}
        self.cnt = {}
        for e in ENGS:
            self.sems["E" + e] = stack.enter_context(nc.semaphore("prog_" + e))
            self.cnt["E" + e] = 0
        self.dma_ring = {}
        for e in ("sp", "pool", "act"):
            keys = []
            for i in range(n_dma_sems):
                k = f"D{e}{i}"
                self.sems[k] = stack.enter_context(nc.semaphore(f"dma_{e}{i}"))
                self.cnt[k] = 0
                keys.append(k)
            self.dma_ring[e] = [keys, 0]
        self.nops = 0
        self.floor = {}
        self.floor_pending = set()

    def init_arena(self, nwords=44032):
        self.big = self.stack.enter_context(self.nc.sbuf_tensor("arena", [128, nwords], F32))
        self.a_words = nwords
        self.a_off = 0
        self.a_mark = 0

    def sb(self, name, shape, dt):
        esz = 2 if dt == BF16 else 4
        n = 1
        for s_ in shape[1:]:
            n *= s_
        words = (n * esz + 3) // 4
        words = (words + 7) // 8 * 8
        assert self.a_off + words <= self.a_words, f"SBUF arena overflow at {name}: {self.a_off}+{words}"
        v = self.big[0:shape[0], self.a_off:self.a_off + words]
        self.a_off += words
        if dt != F32:
            v = v.bitcast(dt)
        v = v[:, 0:n]
        if len(shape) == 3:
            v = v.rearrange("p (a b) -> p a b", a=shape[1])
        elif len(shape) == 4:
            v = v.rearrange("p (a b c) -> p a b c", a=shape[1], b=shape[2])
        return v

    def mark(self):
        self.a_mark = self.a_off

    def reset(self):
        self.a_off = self.a_mark
        self.floor = dict(self.cnt)
        self.floor_pending = set(ENGS)

    def ps(self, name, shape, dt=F32):
        return self.stack.enter_context(self.nc.psum_tensor(name, list(shape), dt))

    def _collect(self, eng, reads, writes, is_dma):
        waits = {}
        if eng in self.floor_pending:
            self.floor_pending.discard(eng)
            waits.update({k: v for k, v in self.floor.items() if v > 0})

        def need(sem, val, src, war=False):
            if src == eng and not is_dma:
                if eng == "pe" or war:
                    return
            if val > waits.get(sem, 0):
                waits[sem] = val

        for b in reads:
            if b.w is not None:
                need(*b.w)
        for b in writes:
            if b.w is not None:
                need(*b.w)
            for sem, (val, src) in b.r.items():
                need(sem, val, src, war=True)
        return waits

    def _commit(self, reads, writes, sem, val, eng):
        for b in reads:
            old = b.r.get(sem)
            if old is None or old[0] < val:
                b.r[sem] = (val, eng)
        for b in writes:
            b.w = (sem, val, eng)
            b.r = {}

    def op(self, eng, fn, reads=(), writes=()):
        waits = self._collect(eng, reads, writes, False)
        sem = "E" + eng
        self.cnt[sem] += 1
        val = self.cnt[sem]
        self.q[eng].append((waits, fn, sem, 1))
        self._commit(reads, writes, sem, val, eng)
        self.nops += 1

    def dma(self, eng, fn, reads=(), writes=()):
        waits = self._collect(eng, reads, writes, True)
        ring = self.dma_ring[eng]
        sem = ring[0][ring[1] % len(ring[0])]
        ring[1] += 1
        prev = self.cnt[sem]
        if prev > waits.get(sem, 0):
            waits[sem] = prev
        self.cnt[sem] += 16
        val = self.cnt[sem]
        self.q[eng].append((waits, fn, sem, 16))
        self._commit(reads, writes, sem, val, eng)
        self.nops += 1

    def emit(self):
        nc = self.nc
        engobj = {"pe": "tensor", "act": "scalar", "dve": "vector", "pool": "gpsimd", "sp": "sync"}
        final = dict(self.cnt)
        with nc.Block() as block:
            for e in ENGS:
                items = self.q[e]

                def body(eo, items=items, e=e):
                    waited = {}
                    for waits, fn, sem, amt in items:
                        for s, v in waits.items():
                            if waited.get(s, 0) < v:
                                eo.wait_ge(self.sems[s], v)
                                waited[s] = v
                        ins = fn(eo)
                        ins.then_inc(self.sems[sem], amt)
                    if e == "sp":
                        for s, v in final.items():
                            if v > 0 and waited.get(s, 0) < v:
                                eo.wait_ge(self.sems[s], v)

                getattr(block, engobj[e])(body)


def bcast_rows(ap_row, n):
    return bass.AP(ap_row.tensor, ap_row.offset, [[0, 128], [1, n]])


class Ctx:
    pass


def setup_ctx(nc, p, cst):
    c = Ctx()
    c.nc, c.p = nc, p
    c.PB = [p.ps(f"pb{i}", [128, 512]) for i in range(8)]
    c.PBb = [Buf(f"pb{i}") for i in range(8)]
    c.id32 = p.sb("id32", [128, 128], F32)
    c.idb = p.sb("idb", [128, 128], BF16)
    c.id_b = Buf("ident")
    p.dma("sp", lambda e: e.dma_start(out=c.id32[:], in_=cst["ident"]), writes=[c.id_b])
    p.dma("pool", lambda e: e.dma_start(out=c.idb[:], in_=cst["ident"]), writes=[c.id_b])
    c.rr = 0
    return c


def evac(c, out, in_, reads, writes, eng=None):
    p = c.p
    if eng is None:
        eng = ("act", "dve")[c.rr % 2]
        c.rr += 1
    if eng == "act":
        p.op("act", lambda e: e.activation(out=out, in_=in_, func=AF.Copy), reads=reads, writes=writes)
    else:
        p.op("dve", lambda e: e.tensor_copy(out=out, in_=in_), reads=reads, writes=writes)


def transpose_tile32(c, src, srcB, dstb, dstB, col0, banks, dst32=None, dst32B=None):
    p = c.p
    for b in range(4):
        bk = banks[b]
        for j in range(4):
            kc = 4 * b + j
            p.op("pe", lambda e, bk=bk, j=j, kc=kc: e.transpose(
                out=c.PB[bk][:, j * 128:(j + 1) * 128], in_=src[:, kc * 128:(kc + 1) * 128], identity=c.id32[:]),
                reads=[srcB, c.id_b], writes=[c.PBb[bk]])
        pv = c.PB[bk][:, :].rearrange("p (a b) -> p a b", a=4)
        evac(c, dstb[:, 4 * b:4 * b + 4, col0:col0 + 128], pv, [c.PBb[bk]], [dstB], eng="act")
        if dst32 is not None:
            evac(c, dst32[:, 4 * b:4 * b + 4, col0:col0 + 128], pv, [c.PBb[bk]], dst32B[4 * b:4 * b + 4], eng="act")


def load_w_block(c, dst, dstB, w, col0, ncols, nk=NKC):
    src = w[:, col0:col0 + ncols].rearrange("(kc p) c -> p kc c", p=128)
    c.p.dma("pool", lambda e: e.dma_start(out=dst, in_=src), writes=[dstB])


def ln_epilogue(c, banks, resid, residB, out_dram, gbc, bbc, lnB, W):
    p = c.p
    st, mv, rs, B2 = W["st"], W["mv"], W["rs"], W["B2"]
    t, tB = resid, residB
    for s in range(4):
        sl = slice(s * 512, (s + 1) * 512)
        p.op("dve", lambda e, s=s, sl=sl: e.scalar_tensor_tensor(
            out=t[:, sl], in0=t[:, sl], scalar=ALPHA, in1=c.PB[banks[s]][:, :], op0=ALU.mult, op1=ALU.add),
            reads=[tB, c.PBb[banks[s]]], writes=[tB])
    for s in range(4):
        p.op("dve", lambda e, s=s: e.bn_stats(out=st[:, s * 6:(s + 1) * 6], in_=t[:, s * 512:(s + 1) * 512]),
             reads=[tB], writes=[B2])
    p.op("dve", lambda e: e.bn_aggr(out=mv, in_=st), reads=[B2], writes=[B2])
    p.op("act", lambda e: e.activation(out=rs, in_=mv[:, 1:2], func=AF.Sqrt, bias=LN_EPS, scale=1.0),
         reads=[B2], writes=[B2])
    p.op("dve", lambda e: e.reciprocal(out=rs, in_=rs), reads=[B2], writes=[B2])
    p.op("dve", lambda e: e.tensor_scalar(out=t, in0=t, scalar1=mv[:, 0:1], scalar2=rs[:, 0:1],
                                          op0=ALU.subtract, op1=ALU.mult), reads=[tB, B2], writes=[tB])
    p.op("pool", lambda e: e.tensor_tensor(out=t, in0=t, in1=gbc, op=ALU.mult), reads=[tB, lnB], writes=[tB])
    p.op("pool", lambda e: e.tensor_tensor(out=t, in0=t, in1=bbc, op=ALU.add), reads=[tB, lnB], writes=[tB])
    p.dma("sp", lambda e: e.dma_start(out=out_dram, in_=t), reads=[tB], writes=[W["outB"]])


def ln_work(p, tag, outB):
    return {"st": p.sb(f"lnst{tag}", [128, 24], F32),
            "mv": p.sb(f"lnmv{tag}", [128, 2], F32), "rs": p.sb(f"lnrs{tag}", [128, 1], F32), "B2": Buf(),
            "outB": outB}


def load_ln(c, g_row, b_row, gbc, bbc, lnB):
    c.p.dma("sp", lambda e: e.dma_start(out=gbc, in_=bcast_rows(g_row, D)), writes=[lnB])
    c.p.dma("sp", lambda e: e.dma_start(out=bbc, in_=bcast_rows(b_row, D)), writes=[lnB])


def gla_phase(c, x_cat, w_in, w_gate_up, b_gate, g_norm, cmask_d, oT_d, first_out=4):
    p = c.p
    p.reset()
    xin = p.sb("xin", [128, D], F32); xinB = Buf()
    xT = p.sb("xT", [128, NKC, G], BF16); xTB = Buf()
    wblk = [p.sb(f"wblk{i}", [128, NKC, 256], BF16) for i in range(2)]; wblkB = [Buf(), Buf()]
    wz = p.sb("wz", [128, NKC, 16], BF16); wzB = Buf()
    wgu = p.sb("wgu", [17, 1024], BF16); wguB = Buf()
    zT = p.sb("zT", [17, G], BF16); zTB = Buf()
    cs = p.sb("cs", [128, 8, G], F32); csB = [Buf() for _ in range(8)]
    dec = p.sb("dec", [128, 8, 4], F32)
    e1 = [p.sb(f"e1_{i}", [128, G], F32) for i in range(2)]; e1B = [Buf(), Buf()]
    ones = p.sb("ones", [128, 128], F32); onesB = Buf()
    qdT = p.sb("qdT", [128, 8, G], BF16); qdB = [Buf() for _ in range(8)]
    kiT = p.sb("kiT", [128, 8, G], BF16); kiB = [Buf() for _ in range(8)]
    keT = p.sb("keT", [128, 8, G], BF16); keB = [Buf() for _ in range(8)]
    v = p.sb("v", [128, 4, D], BF16); vB = [Buf() for _ in range(4)]
    sr = p.sb("sr", [128, 4, D], BF16); srB = [Buf() for _ in range(4)]
    gnbc = p.sb("gnbc", [128, D], F32); gnB = Buf()
    stmp = [p.sb(f"stmp{i}", [128, 256], F32) for i in range(2)]; stmpB = [Buf(), Buf()]
    S = p.sb("S", [128, 8, 512], F32); SB = [Buf() for _ in range(8)]
    Sb = p.sb("Sb", [128, 8, 512], BF16); SbB = [Buf() for _ in range(8)]
    ke = p.sb("ke", [128, 1024], BF16); keTB = Buf()
    attT = p.sb("attT", [128, 4, 128], BF16); attB = Buf()
    cmask = p.sb("cmask", [128, 4, 128], F32); cmB = Buf()
    junk = p.sb("junk", [128, 512], F32); junkB = Buf()
    ss = p.sb("ss", [128, 4], F32); rst = p.sb("rst", [128, 4], F32); ssB = Buf()
    ofin = p.sb("ofin", [128, D], BF16); ofinB = Buf()
    ofT = p.sb("ofT", [128, NKC, 128], BF16); ofTB = Buf()
    outB = Buf()

    p.dma("pool", lambda e: e.dma_start(out=wz, in_=w_in[:, 6144:6160].rearrange("(kc p) c -> p kc c", p=128)), writes=[wzB])
    p.dma("pool", lambda e: e.dma_start(out=wgu[0:16, :], in_=w_gate_up), writes=[wguB])
    p.dma("pool", lambda e: e.dma_start(out=wgu[16:17, :], in_=b_gate), writes=[wguB])
    for h in range(4):
        p.dma("sp", lambda e, h=h: e.dma_start(out=gnbc[:, h * 512:(h + 1) * 512], in_=bcast_rows(g_norm, 512)), writes=[gnB])
    for h in range(4):
        p.dma("sp", lambda e, h=h: e.dma_start(out=cmask[:, h, :], in_=cmask_d), writes=[cmB])
    p.op("pool", lambda e: e.memset(ones, 1.0), writes=[onesB])
    p.op("pool", lambda e: e.memset(zT, 1.0), writes=[zTB])
    p.op("pool", lambda e: e.memset(S.rearrange("p a b -> p (a b)"), 0.0), writes=SB)
    p.op("pool", lambda e: e.memset(Sb.rearrange("p a b -> p (a b)"), 0.0), writes=SbB)

    LNSC = math.log(256 ** -0.5)
    wi = [0]

    def wload(col0):
        i = wi[0] % 2
        wi[0] += 1
        load_w_block(c, wblk[i], wblkB[i], w_in, col0, 256)
        return i

    for g in range(8):
        own = g >= first_out
        xsrc = x_cat
        g4 = g
        for t in range(4):
            r0 = g4 * G + t * 128
            p.dma("sp", lambda e, r0=r0, xsrc=xsrc: e.dma_start(out=xin, in_=xsrc[r0:r0 + 128, :]), writes=[xinB])
            transpose_tile32(c, xin, xinB, xT, xTB, t * 128, (4, 5, 6, 7))
        for kc in range(NKC):
            p.op("pe", lambda e, kc=kc: e.matmul(c.PB[0][0:16, :], lhsT=wz[:, kc, :], rhs=xT[:, kc, :], start=(kc == 0), stop=(kc == NKC - 1)),
                 reads=[wzB, xTB], writes=[c.PBb[0]])
        evac(c, zT[0:16, :], c.PB[0][0:16, :], [c.PBb[0]], [zTB], eng="act")
        for dk in range(8):
            bk = 1 + dk % 2
            p.op("pe", lambda e, dk=dk, bk=bk: e.matmul(c.PB[bk][:, :], lhsT=wgu[:, dk * 128:(dk + 1) * 128], rhs=zT, start=True, stop=True),
                 reads=[wguB, zTB], writes=[c.PBb[bk]])
            a, b = e1[0], e1[1]
            p.op("act", lambda e, bk=bk, a=a: e.activation(out=a, in_=c.PB[bk][:, :], func=AF.Exp, scale=-1.0), reads=[c.PBb[bk]], writes=[e1B[0]])
            p.op("act", lambda e, a=a, b=b: e.activation(out=b, in_=a, func=AF.Ln, bias=1.0, scale=1.0), reads=[e1B[0]], writes=[e1B[1]])
            for ch in range(4):
                p.op("dve", lambda e, dk=dk, ch=ch, b=b: e.tensor_tensor_scan(
                    out=cs[:, dk, ch * 128:(ch + 1) * 128], data0=ones, data1=b[:, ch * 128:(ch + 1) * 128], initial=0.0,
                    op0=ALU.mult, op1=ALU.add), reads=[onesB, e1B[1]], writes=[csB[dk]])
            p.op("act", lambda e, dk=dk: e.activation(
                out=dec[:, dk, :], in_=cs[:, dk, :].rearrange("p (a b) -> p a b", a=4)[:, :, 127], func=AF.Exp, scale=-1.0 / 16.0),
                reads=[csB[dk]], writes=[csB[dk]])
        kinds = [("k", b) for b in range(4)] + ([("q", b) for b in range(4)] if own else [])
        kinds += [("v", b) for b in range(8)] + ([("r", b) for b in range(8)] if own else [])
        base = {"q": 0, "k": 1024, "v": 2048, "r": 4096}
        nxt = wload(base[kinds[0][0]] + kinds[0][1] * 256)
        for bi, (kind, b) in enumerate(kinds):
            cur = nxt
            if bi + 1 < len(kinds):
                nxt = wload(base[kinds[bi + 1][0]] + kinds[bi + 1][1] * 256)
            W = wblk[cur]
            if kind in ("q", "k"):
                for j in range(2):
                    cc = 2 * b + j
                    bk = (2 * bi + j) % 4
                    for kc in range(NKC):
                        p.op("pe", lambda e, W=W, j=j, kc=kc, bk=bk: e.matmul(
                            c.PB[bk][:, :], lhsT=W[:, kc, j * 128:(j + 1) * 128], rhs=xT[:, kc, :], start=(kc == 0), stop=(kc == NKC - 1)),
                            reads=[wblkB[cur], xTB], writes=[c.PBb[bk]])
                    et = e1[0]
                    if kind == "q":
                        p.op("act", lambda e, cc=cc, et=et: e.activation(out=et, in_=cs[:, cc, :], func=AF.Exp, scale=-1.0 / 16.0, bias=LNSC),
                             reads=[csB[cc]], writes=[e1B[0]])
                        p.op("dve", lambda e, cc=cc, bk=bk, et=et: e.tensor_tensor(out=qdT[:, cc, :], in0=c.PB[bk][:, :], in1=et, op=ALU.mult),
                             reads=[c.PBb[bk], e1B[0]], writes=[qdB[cc]])
                    else:
                        p.op("act", lambda e, cc=cc, et=et: e.activation(out=et, in_=cs[:, cc, :], func=AF.Exp, scale=1.0 / 16.0),
                             reads=[csB[cc]], writes=[e1B[0]])
                        p.op("dve", lambda e, cc=cc, bk=bk, et=et: e.tensor_tensor(out=kiT[:, cc, :], in0=c.PB[bk][:, :], in1=et, op=ALU.mult),
                             reads=[c.PBb[bk], e1B[0]], writes=[kiB[cc]])
                        for ch in range(4):
                            sl = slice(ch * 128, (ch + 1) * 128)
                            p.op("dve", lambda e, cc=cc, bk=bk, et=et, ch=ch, sl=sl: e.scalar_tensor_tensor(
                                out=keT[:, cc, sl], in0=c.PB[bk][:, sl], scalar=dec[:, cc, ch:ch + 1], in1=et[:, sl], op0=ALU.mult, op1=ALU.mult),
                                reads=[c.PBb[bk], e1B[0], csB[cc]], writes=[keB[cc]])
            else:
                col0 = b * 256
                for t in range(4):
                    bk = 4 + (t // 2) + 2 * (bi % 2)
                    half = (t % 2) * 256
                    for kc in range(NKC):
                        p.op("pe", lambda e, W=W, t=t, kc=kc, bk=bk, half=half: e.matmul(
                            c.PB[bk][:, half:half + 256], lhsT=xT[:, kc, t * 128:(t + 1) * 128], rhs=W[:, kc, :], start=(kc == 0), stop=(kc == NKC - 1)),
                            reads=[wblkB[cur], xTB], writes=[c.PBb[bk]])
                    if kind == "v":
                        evac(c, v[:, t, col0:col0 + 256], c.PB[bk][:, half:half + 256], [c.PBb[bk]], [vB[t]], eng="act")
                    else:
                        si = t % 2
                        p.op("act", lambda e, bk=bk, half=half, si=si: e.activation(out=stmp[si], in_=c.PB[bk][:, half:half + 256], func=AF.Silu),
                             reads=[c.PBb[bk]], writes=[stmpB[si]])
                        p.op("pool", lambda e, t=t, col0=col0, si=si: e.tensor_tensor(out=sr[:, t, col0:col0 + 256], in0=stmp[si], in1=gnbc[:, col0:col0 + 256], op=ALU.mult),
                             reads=[stmpB[si], gnB], writes=[srB[t]])
        for ch in range(4):
            sl = slice(ch * 128, (ch + 1) * 128)
            if own:
                for h in range(4):
                    for tcl in range(2):
                        tc = 2 * h + tcl
                        p.op("pe", lambda e, h=h, tc=tc, tcl=tcl, sl=sl: e.matmul(
                            c.PB[4][:, h * 128:(h + 1) * 128], lhsT=kiT[:, tc, sl], rhs=qdT[:, tc, sl], start=(tcl == 0), stop=(tcl == 1)),
                            reads=[kiB[tc], qdB[tc]], writes=[c.PBb[4]])
                p.op("dve", lambda e: e.tensor_tensor(out=attT, in0=c.PB[4][:, :].rearrange("p (a b) -> p a b", a=4), in1=cmask, op=ALU.mult),
                     reads=[c.PBb[4], cmB], writes=[attB])
                for h in range(4):
                    p.op("pe", lambda e, h=h, ch=ch: e.matmul(c.PB[h][:, :], lhsT=attT[:, h, :], rhs=v[:, ch, h * 512:(h + 1) * 512], start=True, stop=False),
                         reads=[attB, vB[ch]], writes=[c.PBb[h]])
                    for tcl in range(2):
                        tc = 2 * h + tcl
                        p.op("pe", lambda e, h=h, tc=tc, tcl=tcl, sl=sl: e.matmul(c.PB[h][:, :], lhsT=qdT[:, tc, sl], rhs=Sb[:, tc, :], start=False, stop=(tcl == 1)),
                             reads=[qdB[tc], SbB[tc]], writes=[c.PBb[h]])
            pb7 = c.PB[7].bitcast(BF16)
            for tc in range(8):
                p.op("pe", lambda e, tc=tc, sl=sl: e.transpose(out=pb7[:, tc * 128:(tc + 1) * 128], in_=keT[:, tc, sl], identity=c.idb),
                     reads=[keB[tc], c.id_b], writes=[c.PBb[7]])
            evac(c, ke, pb7[:, 0:1024], [c.PBb[7]], [keTB], eng="act")
            for tc in range(8):
                h = tc // 2
                bk = 5 + tc % 2
                p.op("pe", lambda e, tc=tc, h=h, bk=bk, ch=ch: e.matmul(c.PB[bk][:, :], lhsT=ke[:, tc * 128:(tc + 1) * 128], rhs=v[:, ch, h * 512:(h + 1) * 512], start=True, stop=True),
                     reads=[keTB, vB[ch]], writes=[c.PBb[bk]])
                p.op("dve", lambda e, tc=tc, bk=bk, ch=ch: e.scalar_tensor_tensor(
                    out=S[:, tc, :], in0=S[:, tc, :], scalar=dec[:, tc, ch:ch + 1], in1=c.PB[bk][:, :], op0=ALU.mult, op1=ALU.add),
                    reads=[SB[tc], c.PBb[bk], csB[tc]], writes=[SB[tc]])
                p.op("pool", lambda e, tc=tc: e.tensor_copy(out=Sb[:, tc, :], in_=S[:, tc, :]), reads=[SB[tc]], writes=[SbB[tc]])
            if own:
                for h in range(4):
                    p.op("act", lambda e, h=h: e.activation(out=junk, in_=c.PB[h][:, :], func=AF.Square, accum_out=ss[:, h:h + 1]),
                         reads=[c.PBb[h]], writes=[junkB, ssB])
                p.op("act", lambda e: e.activation(out=rst, in_=ss, func=AF.Sqrt, scale=1.0 / 512.0, bias=RMS_EPS), reads=[ssB], writes=[ssB])
                p.op("dve", lambda e: e.reciprocal(out=rst, in_=rst), reads=[ssB], writes=[ssB])
                for h in range(4):
                    hs = slice(h * 512, (h + 1) * 512)
                    p.op("dve", lambda e, h=h, hs=hs, ch=ch: e.scalar_tensor_tensor(
                        out=ofin[:, hs], in0=c.PB[h][:, :], scalar=rst[:, h:h + 1], in1=sr[:, ch, hs], op0=ALU.mult, op1=ALU.mult),
                        reads=[c.PBb[h], ssB, srB[ch]], writes=[ofinB])
                for half in range(2):
                    bk = 5 + half
                    pbv = c.PB[bk].bitcast(BF16)
                    for j in range(8):
                        kc = half * 8 + j
                        p.op("pe", lambda e, pbv=pbv, j=j, kc=kc: e.transpose(out=pbv[:, j * 128:(j + 1) * 128], in_=ofin[:, kc * 128:(kc + 1) * 128], identity=c.idb),
                             reads=[ofinB, c.id_b], writes=[c.PBb[bk]])
                    evac(c, ofT[:, half * 8:half * 8 + 8, :], pbv[:, 0:1024].rearrange("p (a b) -> p a b", a=8), [c.PBb[bk]], [ofTB], eng="act")
                tile = (g - first_out) * 4 + ch
                p.dma("sp", lambda e, tile=tile: e.dma_start(out=oT_d[tile], in_=ofT), reads=[ofTB], writes=[outB])


def proj_ln_phase(c, aT_d, w, resid_d, g_row, b_row, out_d, ntiles=T // 128):
    p = c.p
    p.reset()
    W = p.sb("W", [128, NKC, D], BF16); WB = [Buf() for _ in range(4)]
    aT = [p.sb(f"aT{i}", [128, NKC, 128], BF16) for i in range(2)]; aTB = [Buf(), Buf()]
    res = [p.sb(f"res{i}", [128, D], F32) for i in range(2)]; resB = [Buf(), Buf()]
    gbc = p.sb("gbc", [128, D], F32); bbc = p.sb("bbc", [128, D], F32); lnB = Buf()
    outB = Buf()
    lw = [ln_work(p, i, outB) for i in range(2)]
    for s in range(4):
        load_w_block(c, W[:, :, s * 512:(s + 1) * 512], WB[s], w, s * 512, 512)
    load_ln(c, g_row, b_row, gbc, bbc, lnB)
    for t in range(ntiles):
        i = t % 2
        p.dma("sp", lambda e, t=t, i=i: e.dma_start(out=aT[i], in_=aT_d[t]), writes=[aTB[i]])
        p.dma("sp", lambda e, t=t, i=i: e.dma_start(out=res[i], in_=resid_d[t * 128:(t + 1) * 128, :]), writes=[resB[i]])
        banks = [4 * i + s for s in range(4)]
        for s in range(4):
            for kc in range(NKC):
                p.op("pe", lambda e, s=s, kc=kc, i=i, bk=banks[s]: e.matmul(
                    c.PB[bk][:, :], lhsT=aT[i][:, kc, :], rhs=W[:, kc, s * 512:(s + 1) * 512], start=(kc == 0), stop=(kc == NKC - 1)),
                    reads=[aTB[i], WB[s]], writes=[c.PBb[banks[s]]])
        ln_epilogue(c, banks, res[i], resB[i], out_d[t * 128:(t + 1) * 128, :], gbc, bbc, lnB, lw[i])


MOEDBG = 0


def ffn_phase(c, h_d, w_gu_list, w_d_list, F, g_row, b_row, out_d, w_router=None, esel_d=None, ntok=T):
    p = c.p
    p.reset()
    moe = w_router is not None
    nfc = F // 128
    hin = [p.sb("hin0", [128, D], F32)] * 2; hinB = [Buf()] * 2
    hT = p.sb("hT", [128, NKC, G], BF16); hTB = Buf()
    big32 = p.sb("big32", [128, NKC, G], F32); bigB = [Buf() for _ in range(NKC)]
    NQ = 4
    qb_ = [(nfc * q_) // NQ for q_ in range(NQ + 1)]
    maxq = max(qb_[q_ + 1] - qb_[q_] for q_ in range(NQ))
    NWB = 4
    wg = [p.sb(f"wg{i}", [128, NKC, 128], BF16) for i in range(NWB)]; wgB = [Buf() for _ in range(NWB)]
    wu = [p.sb(f"wu{i}", [128, NKC, 128], BF16) for i in range(NWB)]; wuB = [Buf() for _ in range(NWB)]
    wd = [p.sb(f"wd{i}", [128, maxq, 128], BF16) for i in range(NWB)]; wdB = [Buf() for _ in range(NWB)]
    actT = p.sb("actT", [128, maxq, G], BF16); actB = [Buf() for _ in range(maxq)]
    sg = [p.sb("sg0", [128, G], F32)] * 2; sgB = [Buf()] * 2
    gbc = p.sb("gbc", [128, D], F32); bbc = p.sb("bbc", [128, D], F32); lnB = Buf()
    outB = Buf()
    lw = [ln_work(p, 0, outB)]
    load_ln(c, g_row, b_row, gbc, bbc, lnB)
    if moe:
        wr = p.sb("wr", [128, NKC, NEXP], F32); wrB = Buf()
        esel = p.sb("esel", [8, NEXP, 128], F32); eselB = Buf()
        p.dma("sp", lambda e: e.dma_start(out=wr, in_=w_router.rearrange("(kc p) c -> p kc c", p=128)), writes=[wrB])
        p.dma("sp", lambda e: e.dma_start(out=esel, in_=esel_d), writes=[eselB])
        lg = p.sb("lg", [128, 8], F32); mx = p.sb("mx", [128, 8], F32); el = p.sb("el", [128, 8], F32)
        msk = p.sb("msk", [128, 8], F32); sm = p.sb("sm", [128, 4], F32); gts = p.sb("gts", [128, 8], F32); rB = Buf()
        gT = p.sb("gT", [8, G], F32); gTB = Buf()
        gb = p.sb("gb", [128, G], F32); gbB = Buf()
        tmp = [p.sb(f"tmp{i}", [128, G], F32) for i in range(2)]; tmpB = [Buf(), Buf()]
    n_exp = len(w_gu_list)
    for g in range(ntok // G):
        for t in range(4):
            i = t % 2
            r0 = g * G + t * 128
            p.dma("sp", lambda e, r0=r0, i=i: e.dma_start(out=hin[i], in_=h_d[r0:r0 + 128, :]), writes=[hinB[i]])
            transpose_tile32(c, hin[i], hinB[i], hT, hTB, t * 128, (4, 5, 6, 7), dst32=big32 if (moe and not (MOEDBG & 8)) else None,
                             dst32B=bigB)
        if moe:
            for t in range(4):
                if MOEDBG & 1:
                    p.op("dve", lambda e: e.tensor_copy(out=lg, in_=wr[:, 0, :]), reads=[wrB], writes=[rB])
                else:
                    for kc in range(NKC):
                        p.op("pe", lambda e, t=t, kc=kc: e.matmul(c.PB[0][:, 0:8], lhsT=big32[:, kc, t * 128:(t + 1) * 128], rhs=wr[:, kc, :],
                                                                  start=(kc == 0), stop=(kc == NKC - 1)),
                             reads=[bigB[kc], wrB], writes=[c.PBb[0]])
                    p.op("dve", lambda e: e.tensor_copy(out=lg, in_=c.PB[0][:, 0:8]), reads=[c.PBb[0]], writes=[rB])
                p.op("dve", lambda e: e.max(out=mx, in_=lg), reads=[rB], writes=[rB])
                p.op("dve", lambda e: e.tensor_scalar(out=sm[:, 0:1], in0=mx[:, 0:1], scalar1=-1.0, scalar2=None, op0=ALU.mult), reads=[rB], writes=[rB])
                p.op("act", lambda e: e.activation(out=el, in_=lg, func=AF.Exp, bias=sm[:, 0:1], scale=1.0), reads=[rB], writes=[rB])
                p.op("act", lambda e: e.activation(out=sm[:, 1:2], in_=mx[:, 1:2], func=AF.Exp, bias=sm[:, 0:1], scale=1.0), reads=[rB], writes=[rB])
                p.op("dve", lambda e: e.tensor_scalar(out=sm[:, 2:3], in0=sm[:, 1:2], scalar1=1.0, scalar2=None, op0=ALU.add), reads=[rB], writes=[rB])
                p.op("dve", lambda e: e.reciprocal(out=sm[:, 3:4], in_=sm[:, 2:3]), reads=[rB], writes=[rB])
                p.op("dve", lambda e: e.tensor_scalar(out=msk, in0=lg, scalar1=mx[:, 1:2], scalar2=None, op0=ALU.is_ge), reads=[rB], writes=[rB])
                p.op("dve", lambda e: e.scalar_tensor_tensor(out=gts, in0=el, scalar=sm[:, 3:4], in1=msk, op0=ALU.mult, op1=ALU.mult), reads=[rB], writes=[rB])
                if not (MOEDBG & 2):
                    p.op("pe", lambda e, t=t: e.transpose(out=c.PB[1][0:8, t * 128:(t + 1) * 128], in_=gts, identity=c.id32), reads=[rB, c.id_b], writes=[c.PBb[1]])
            if MOEDBG & 2:
                p.op("dve", lambda e: e.memset(gT, 0.5), reads=[rB], writes=[gTB])
            else:
                evac(c, gT, c.PB[1][0:8, :], [c.PBb[1]], [gTB], eng="dve")
        if g == 0:
            schedU = [(ex, j) for ex in range(n_exp) for q_ in range(NQ) for j in range(qb_[q_], qb_[q_ + 1])]
            schedD = [(ex, q_, i_) for ex in range(n_exp) for q_ in range(NQ) for i_ in range(NKC)]
            st_ = {"iu": 0, "id": 0, "nu": 0, "nd": 0}

            def ensure():
                while st_["iu"] < min(st_["nu"] + NWB - 1, len(schedU) * (ntok // G)):
                    ex_, j_ = schedU[st_["iu"] % len(schedU)]
                    k_ = st_["iu"] % NWB
                    load_w_block(c, wg[k_], wgB[k_], w_gu_list[ex_], j_ * 128, 128)
                    load_w_block(c, wu[k_], wuB[k_], w_gu_list[ex_], F + j_ * 128, 128)
                    st_["iu"] += 1
                while st_["id"] < min(st_["nd"] + NWB - 1, len(schedD) * (ntok // G)):
                    ex_, q2, i2 = schedD[st_["id"] % len(schedD)]
                    k_ = st_["id"] % NWB
                    f0, f1 = qb_[q2], qb_[q2 + 1]
                    src = w_d_list[ex_][f0 * 128:f1 * 128, i2 * 128:(i2 + 1) * 128].rearrange("(fc p) c -> p fc c", p=128)
                    p.dma("pool", lambda e, k_=k_, src=src, n_=f1 - f0: e.dma_start(out=wd[k_][:, 0:n_, :], in_=src), writes=[wdB[k_]])
                    st_["id"] += 1
        for ex in range(n_exp):
            if moe:
                p.op("pe", lambda e, ex=ex: e.matmul(c.PB[0][:, :], lhsT=esel[:, ex, :], rhs=gT, start=True, stop=True), reads=[eselB, gTB], writes=[c.PBb[0]])
                evac(c, gb, c.PB[0][:, :], [c.PBb[0]], [gbB], eng="act")
            for q_ in range(NQ):
                f0, f1 = qb_[q_], qb_[q_ + 1]
                for j in range(f0, f1):
                    st_["nu"] += 1
                    ensure()
                    cur = (st_["nu"] - 1) % NWB
                    bg, bu = (j % 2) * 2, (j % 2) * 2 + 1
                    for kc in range(NKC):
                        p.op("pe", lambda e, kc=kc, cur=cur, bg=bg: e.matmul(c.PB[bg][:, :], lhsT=wg[cur][:, kc, :], rhs=hT[:, kc, :], start=(kc == 0), stop=(kc == NKC - 1)),
                             reads=[wgB[cur], hTB], writes=[c.PBb[bg]])
                    for kc in range(NKC):
                        p.op("pe", lambda e, kc=kc, cur=cur, bu=bu: e.matmul(c.PB[bu][:, :], lhsT=wu[cur][:, kc, :], rhs=hT[:, kc, :], start=(kc == 0), stop=(kc == NKC - 1)),
                             reads=[wuB[cur], hTB], writes=[c.PBb[bu]])
                    si = j % 2
                    p.op("act", lambda e, bg=bg, si=si: e.activation(out=sg[si], in_=c.PB[bg][:, :], func=AF.Silu), reads=[c.PBb[bg]], writes=[sgB[si]])
                    p.op("dve", lambda e, bu=bu, si=si, j=j, f0=f0: e.tensor_tensor(out=actT[:, j - f0, :], in0=sg[si], in1=c.PB[bu][:, :], op=ALU.mult),
                         reads=[sgB[si], c.PBb[bu]], writes=[actB[j - f0]])
                for i_ in range(NKC):
                    st_["nd"] += 1
                    ensure()
                    cur = (st_["nd"] - 1) % NWB
                    bk = 4 + i_ % 2
                    for fc in range(f0, f1):
                        p.op("pe", lambda e, fc=fc, f0=f0, f1=f1, cur=cur, bk=bk: e.matmul(c.PB[bk][:, :], lhsT=wd[cur][:, fc - f0, :], rhs=actT[:, fc - f0, :], start=(fc == f0), stop=(fc == f1 - 1)),
                             reads=[wdB[cur], actB[fc - f0]], writes=[c.PBb[bk]])
                    first = (ex == 0 and q_ == 0)
                    if not moe:
                        if first:
                            evac(c, big32[:, i_, :], c.PB[bk][:, :], [c.PBb[bk]], [bigB[i_]])
                        else:
                            p.op("dve", lambda e, i_=i_, bk=bk: e.tensor_tensor(out=big32[:, i_, :], in0=big32[:, i_, :], in1=c.PB[bk][:, :], op=ALU.add),
                                 reads=[c.PBb[bk], bigB[i_]], writes=[bigB[i_]])
                    elif first:
                        p.op("dve", lambda e, i_=i_, bk=bk: e.tensor_tensor(out=big32[:, i_, :], in0=c.PB[bk][:, :], in1=gb, op=ALU.mult),
                             reads=[c.PBb[bk], gbB], writes=[bigB[i_]])
                    else:
                        ti = i_ % 2
                        p.op("dve", lambda e, bk=bk, ti=ti: e.tensor_tensor(out=tmp[ti], in0=c.PB[bk][:, :], in1=gb, op=ALU.mult),
                             reads=[c.PBb[bk], gbB], writes=[tmpB[ti]])
                        p.op("pool", lambda e, i_=i_, ti=ti: e.tensor_tensor(out=big32[:, i_, :], in0=big32[:, i_, :], in1=tmp[ti], op=ALU.add),
                             reads=[tmpB[ti], bigB[i_]], writes=[bigB[i_]])
        for t in range(4):
            r0 = g * G + t * 128
            i = t % 2
            p.dma("sp", lambda e, r0=r0, i=i: e.dma_start(out=hin[i], in_=h_d[r0:r0 + 128, :]), writes=[hinB[i]])
            for i_ in range(NKC):
                bk = i_ // 4
                p.op("pe", lambda e, i_=i_, bk=bk, t=t: e.transpose(out=c.PB[bk][:, (i_ % 4) * 128:(i_ % 4 + 1) * 128], in_=big32[:, i_, t * 128:(t + 1) * 128], identity=c.id32),
                     reads=[bigB[i_], c.id_b], writes=[c.PBb[bk]])
            ln_epilogue(c, [0, 1, 2, 3], hin[i], hinB[i], out_d[r0:r0 + 128, :], gbc, bbc, lnB, lw[0])


KVDBG = 1


def kv_phase(c, h_d, w_kv, KT_d, V_d, km_d, ntok=T):
    p = c.p
    p.reset()
    hin = [p.sb(f"hin{i}", [128, D], F32) for i in range(2)]; hinB = [Buf(), Buf()]
    hT = p.sb("hT", [128, NKC, G], BF16); hTB = Buf()
    wkv = p.sb("wkv", [128, NKC, 1024], BF16); wkvB = [Buf(), Buf()]
    KTs = [p.sb(f"KTs{i}", [128, G], BF16) for i in range(2)]; KTsB = [Buf(), Buf()]
    Vs = [p.sb(f"Vs{i}", [128, 512], BF16) for i in range(2)]; VsB = [Buf(), Buf()]
    kms = p.sb("kms", [128, 4, ntok // 256], F32); kmB = Buf()
    outB = Buf()
    for s_ in range(2):
        load_w_block(c, wkv[:, :, s_ * 512:(s_ + 1) * 512], wkvB[s_], w_kv, s_ * 512, 512)
    n = 0
    for g in range(ntok // G):
        for t in range(4):
            i = t % 2
            r0 = g * G + t * 128
            p.dma("sp", lambda e, r0=r0, i=i: e.dma_start(out=hin[i], in_=h_d[r0:r0 + 128, :]), writes=[hinB[i]])
            transpose_tile32(c, hin[i], hinB[i], hT, hTB, t * 128, (4, 5, 6, 7))
        for hd in range(4):
            bk = hd % 2
            for kc in range(NKC):
                p.op("pe", lambda e, hd=hd, kc=kc, bk=bk: e.matmul(c.PB[bk][:, :], lhsT=wkv[:, kc, hd * 128:(hd + 1) * 128], rhs=hT[:, kc, :],
                                                                   start=(kc == 0), stop=(kc == NKC - 1)), reads=[wkvB[0], hTB], writes=[c.PBb[bk]])
            i = n % 2; n += 1
            for hf in range(2):
                p.op("act", lambda e, hd=hd, g=g, bk=bk, hf=hf, i=i: e.activation(
                    out=KTs[i][:, hf * 256:(hf + 1) * 256], in_=c.PB[bk][:, hf * 256:(hf + 1) * 256], func=AF.Copy,
                    accum_out=kms[:, hd, 2 * g + hf:2 * g + hf + 1]), reads=[c.PBb[bk]], writes=[KTsB[i], kmB])
            p.dma("sp", lambda e, hd=hd, g=g, i=i: e.dma_start(out=KT_d[:, hd, g * G:(g + 1) * G], in_=KTs[i]), reads=[KTsB[i]], writes=[outB])
        for t in range(4):
            bk = 2 + t % 2
            for kc in range(NKC):
                p.op("pe", lambda e, t=t, kc=kc, bk=bk: e.matmul(c.PB[bk][:, :], lhsT=hT[:, kc, t * 128:(t + 1) * 128], rhs=wkv[:, kc, 512:1024],
                                                                 start=(kc == 0), stop=(kc == NKC - 1)), reads=[wkvB[1], hTB], writes=[c.PBb[bk]])
            i = t % 2
            evac(c, Vs[i], c.PB[bk][:, :], [c.PBb[bk]], [VsB[i]], eng="dve")
            p.dma("sp", lambda e, t=t, g=g, i=i: e.dma_start(out=V_d[g * 4 + t], in_=Vs[i]), reads=[VsB[i]], writes=[outB])
    p.op("dve", lambda e: e.tensor_scalar(out=kms.rearrange("p a b -> p (a b)"), in0=kms.rearrange("p a b -> p (a b)"), scalar1=1.0 / 256.0, scalar2=None, op0=ALU.mult),
         reads=[kmB], writes=[kmB])
    p.dma("sp", lambda e: e.dma_start(out=km_d, in_=kms), reads=[kmB], writes=[outB])


def bc_last(ap, n):
    d = [list(x) for x in ap.ap]
    return bass.AP(ap.tensor, ap.offset, d + [[0, n]])


def bc_mid(ap, n):
    d = [list(x) for x in ap.ap]
    return bass.AP(ap.tensor, ap.offset, [d[0], [0, n]] + d[1:])


def moba_phase(c, h_d, w_q, KT_all, V_all, km_all, gbias_d, cmask_d, aT_d):
    p = c.p
    p.reset()
    KT = p.sb("KT", [128, 4, 2 * T], BF16); KTB = Buf()
    Va = p.sb("Va", [128, 32, 4, 129], BF16); VaB = Buf()
    kmb = p.sb("kmb", [128, 4, 16], BF16); kmbB = Buf()
    gbs = p.sb("gbs", [128, 8, 16], F32); gbsB = Buf()
    cmask = p.sb("cmask", [128, 4, 128], F32); cmB = Buf()
    hin = [p.sb(f"hin{i}", [128, D], F32) for i in range(2)]; hinB = [Buf(), Buf()]
    hT = p.sb("hT", [128, NKC, G], BF16); hTB = Buf()
    wqb = [p.sb(f"wqb{i}", [128, NKC, 256], BF16) for i in range(2)]; wqbB = [Buf(), Buf()]
    qT = p.sb("qT", [128, 16, G], BF16); qTB = [Buf() for _ in range(16)]
    gm = p.sb("gm", [128, 16, 16], F32); mx8 = p.sb("mx8", [128, 16, 8], F32)
    sel = p.sb("sel", [128, 16, 16], F32); vld = p.sb("vld", [128, 16, 16], F32); selB = Buf()
    acc = p.sb("acc", [128, 16, 129], F32); accB = [Buf() for _ in range(16)]
    PT = [[p.sb(f"PT{i}{j}", [128, 4, 128], BF16) for j in range(2)] for i in range(2)]
    PTB = [[Buf(), Buf()], [Buf(), Buf()]]
    rec = p.sb("rec", [128, 16], F32); recB = Buf()
    attn = p.sb("attn", [128, D], BF16); attnB = Buf()
    aT = p.sb("aT", [128, NKC, 128], BF16); aTB = Buf()
    outB = Buf()
    for hd in range(4):
        p.dma("sp", lambda e, hd=hd: e.dma_start(out=KT[:, hd, :], in_=KT_all[:, hd, :]), writes=[KTB])
    p.op("pool", lambda e: e.memset(Va.rearrange("p a b c -> p (a b c)"), 1.0), writes=[VaB])
    for kt in range(32):
        p.dma("sp", lambda e, kt=kt: e.dma_start(out=Va[:, kt, :, 0:128], in_=V_all[kt].rearrange("p (h d) -> p h d", h=4)), writes=[VaB])
    p.dma("pool", lambda e: e.dma_start(out=kmb, in_=km_all), writes=[kmbB])
    p.dma("sp", lambda e: e.dma_start(out=gbs.rearrange("p a b -> p (a b)"), in_=bcast_rows(gbias_d, 128)), writes=[gbsB])
    for h in range(4):
        p.dma("sp", lambda e, h=h: e.dma_start(out=cmask[:, h, :], in_=cmask_d), writes=[cmB])
    wi = [0]
    QS = 128 ** -0.5
    blkn = [0]
    for g in range(T // G):
        for t in range(4):
            i = t % 2
            r0 = g * G + t * 128
            p.dma("sp", lambda e, r0=r0, i=i: e.dma_start(out=hin[i], in_=h_d[r0:r0 + 128, :]), writes=[hinB[i]])
            transpose_tile32(c, hin[i], hinB[i], hT, hTB, t * 128, (4, 5, 6, 7))
        def wl(b):
            i = wi[0] % 2
            wi[0] += 1
            load_w_block(c, wqb[i], wqbB[i], w_q, b * 256, 256)
            return i
        nxt = wl(0)
        for b in range(8):
            cur = nxt
            if b + 1 < 8:
                nxt = wl(b + 1)
            for j in range(2):
                h = 2 * b + j
                bk = h % 4
                for kc in range(NKC):
                    p.op("pe", lambda e, cur=cur, j=j, kc=kc, bk=bk: e.matmul(c.PB[bk][:, :], lhsT=wqb[cur][:, kc, j * 128:(j + 1) * 128], rhs=hT[:, kc, :],
                                                                            start=(kc == 0), stop=(kc == NKC - 1)), reads=[wqbB[cur], hTB], writes=[c.PBb[bk]])
                p.op("act", lambda e, h=h, bk=bk: e.activation(out=qT[:, h, :], in_=c.PB[bk][:, :], func=AF.Copy, scale=QS), reads=[c.PBb[bk]], writes=[qTB[h]])
        for t in range(4):
            ti = g * 4 + t
            qb = ti // 2
            tw = ti % 2
            ts_ = slice(t * 128, (t + 1) * 128)
            for h in range(16):
                p.op("pe", lambda e, h=h, ts_=ts_: e.matmul(c.PB[0][:, h * 16:(h + 1) * 16], lhsT=qT[:, h, ts_], rhs=kmb[:, h // 4, :], start=True, stop=True),
                     reads=[qTB[h], kmbB], writes=[c.PBb[0]])
            p.op("dve", lambda e, qb=qb: e.tensor_tensor(out=gm, in0=c.PB[0][:, 0:256].rearrange("p (a b) -> p a b", a=16), in1=bc_mid(gbs[:, qb, :], 16), op=ALU.add),
                 reads=[c.PBb[0], gbsB], writes=[selB])
            for h in range(16):
                p.op("dve", lambda e, h=h: e.max(out=mx8[:, h, :], in_=gm[:, h, :]), reads=[selB], writes=[selB])
            p.op("dve", lambda e: e.tensor_tensor(out=sel, in0=gm, in1=bc_last(mx8[:, :, 2], 16), op=ALU.is_ge), reads=[selB], writes=[selB])
            p.op("dve", lambda e: e.tensor_scalar(out=vld.rearrange("p a b -> p (a b)"), in0=gm.rearrange("p a b -> p (a b)"), scalar1=-1e29, scalar2=None, op0=ALU.is_gt),
                 reads=[selB], writes=[selB])
            p.op("dve", lambda e: e.tensor_tensor(out=sel, in0=sel, in1=vld, op=ALU.mult), reads=[selB], writes=[selB])
            slots = list(range(8)) + [8 + n_ for n_ in range(qb + 1)]
            for gh in range(4):
                for si, s_ in enumerate(slots):
                    diag = (s_ == 8 + qb)
                    kb = s_ * 256 if s_ < 8 else T + (s_ - 8) * 256
                    chunks = [0, 1]
                    if diag and tw == 0:
                        chunks = [0]
                    bi = blkn[0] % 2
                    blkn[0] += 1
                    for ck in chunks:
                        bk = 2 * bi + ck
                        k0 = kb + ck * 128
                        p.op("pe", lambda e, gh=gh, k0=k0, bk=bk, ts_=ts_: e.matmul(c.PB[bk][:, :].rearrange("p (a b) -> p a b", a=4), lhsT=KT[:, gh, k0:k0 + 128], rhs=qT[:, 4 * gh:4 * gh + 4, ts_], start=True, stop=True),
                             reads=[KTB] + qTB[4 * gh:4 * gh + 4], writes=[c.PBb[bk]])
                        pt = PT[bi][ck]
                        p.op("act", lambda e, bk=bk, pt=pt: e.activation(out=pt.rearrange("p a b -> p (a b)"), in_=c.PB[bk][:, :], func=AF.Exp), reads=[c.PBb[bk]], writes=[PTB[bi][ck]])
                        if diag and ck == tw:
                            p.op("dve", lambda e, pt=pt: e.tensor_tensor(out=pt, in0=pt, in1=cmask, op=ALU.mult), reads=[PTB[bi][ck], cmB], writes=[PTB[bi][ck]])
                    for hh in range(4):
                        bk = 4 + 2 * bi + hh // 2
                        cs_ = (hh % 2) * 129
                        for ci, ck in enumerate(chunks):
                            kt = (kb + ck * 128) // 128
                            p.op("pe", lambda e, bk=bk, cs_=cs_, hh=hh, ck=ck, kt=kt, gh=gh, bi=bi, ci=ci, nch=len(chunks): e.matmul(
                                c.PB[bk][:, cs_:cs_ + 129], lhsT=PT[bi][ck][:, hh, :], rhs=Va[:, kt, gh, :], start=(ci == 0), stop=(ci == nch - 1)),
                                reads=[PTB[bi][ck], VaB], writes=[c.PBb[bk]])
                    for hh in range(4):
                        h = 4 * gh + hh
                        bk = 4 + 2 * bi + hh // 2
                        cs_ = (hh % 2) * 129
                        if diag:
                            p.op("dve", lambda e, h=h, bk=bk, cs_=cs_: e.tensor_tensor(out=acc[:, h, :], in0=acc[:, h, :], in1=c.PB[bk][:, cs_:cs_ + 129], op=ALU.add),
                                 reads=[accB[h], c.PBb[bk]], writes=[accB[h]])
                        elif si == 0:
                            p.op("dve", lambda e, h=h, bk=bk, cs_=cs_, s_=s_: e.tensor_scalar(out=acc[:, h, :], in0=c.PB[bk][:, cs_:cs_ + 129], scalar1=sel[:, h, s_:s_ + 1], scalar2=None, op0=ALU.mult),
                                 reads=[selB, c.PBb[bk]], writes=[accB[h]])
                        else:
                            p.op("dve", lambda e, h=h, bk=bk, cs_=cs_, s_=s_: e.scalar_tensor_tensor(out=acc[:, h, :], in0=c.PB[bk][:, cs_:cs_ + 129], scalar=sel[:, h, s_:s_ + 1], in1=acc[:, h, :], op0=ALU.mult, op1=ALU.add),
                                 reads=[selB, c.PBb[bk], accB[h]], writes=[accB[h]])
            p.op("dve", lambda e: e.reciprocal(out=rec, in_=acc[:, :, 128]), reads=accB, writes=[recB])
            p.op("dve", lambda e: e.tensor_tensor(out=attn.rearrange("p (a b) -> p a b", a=16), in0=acc[:, :, 0:128], in1=bc_last(rec, 128), op=ALU.mult),
                 reads=accB + [recB], writes=[attnB])
            for half in range(2):
                bk = 2 + half
                pbv = c.PB[bk].bitcast(BF16)
                for j in range(8):
                    kc = half * 8 + j
                    p.op("pe", lambda e, pbv=pbv, j=j, kc=kc: e.transpose(out=pbv[:, j * 128:(j + 1) * 128], in_=attn[:, kc * 128:(kc + 1) * 128], identity=c.idb),
                         reads=[attnB, c.id_b], writes=[c.PBb[bk]])
                evac(c, aT[:, half * 8:half * 8 + 8, :], pbv[:, 0:1024].rearrange("p (a b) -> p a b", a=8), [c.PBb[bk]], [aTB], eng="act")
            p.dma("sp", lambda e, ti=ti: e.dma_start(out=aT_d[ti], in_=aT), reads=[aTB], writes=[outB])


def dram_in(nc, name, shape, dt=F32):
    return nc.dram_tensor(name, list(shape), dt, kind="ExternalInput").ap()


def dram_out(nc, name, shape, dt=F32):
    return nc.dram_tensor(name, list(shape), dt, kind="ExternalOutput").ap()


def build(mode="L1"):
    nc = bass.Bass("TRN2", target_bir_lowering=False)
    cst = {"ident": dram_in(nc, "ident", [128, 128]), "cmask": dram_in(nc, "cmask", [128, 128])}
    with contextlib.ExitStack() as st:
        p = Prog(nc, st)
        p.init_arena()
        c = setup_ctx(nc, p, cst)
        p.mark()
        ln_mix_g = dram_in(nc, "ln_mix_g", [2, D]); ln_mix_b = dram_in(nc, "ln_mix_b", [2, D])
        ln_ffn_g = dram_in(nc, "ln_ffn_g", [2, D]); ln_ffn_b = dram_in(nc, "ln_ffn_b", [2, D])
        aT_d = nc.dram_tensor("aT_d", [2 * T // 128, 128, NKC, 128], BF16, kind="Internal").ap()
        if mode == "F":
            x_cat = dram_in(nc, "x_cat", [2 * T, D])
            w_in = dram_in(nc, "w_in", [D, 6160]); w_gate_up = dram_in(nc, "w_gate_up", [16, 1024])
            b_gate = dram_in(nc, "b_gate", [1, 1024]); g_norm = dram_in(nc, "g_norm", [1, 512])
            w_o_a = dram_in(nc, "w_o_a", [D, D])
            w_gu_dense = dram_in(nc, "w_gu_dense", [D, 2 * F_DENSE]); w_down_dense = dram_in(nc, "w_down_dense", [F_DENSE, D])
            w_kv = dram_in(nc, "w_kv", [D, 1024])
            gbias = dram_in(nc, "gbias", [1, 128]); esel = dram_in(nc, "esel", [8, NEXP, 128])
            w_q = dram_in(nc, "w_q", [D, D]); w_o_b = dram_in(nc, "w_o_b", [D, D])
            w_router = dram_in(nc, "w_router", [D, NEXP])
            w_gu_moe = dram_in(nc, "w_gu_moe", [NEXP, D, 2 * F_EXP]); w_down_moe = dram_in(nc, "w_down_moe", [NEXP, F_EXP, D])
            out = dram_out(nc, "out", [T, D])
            hm0 = nc.dram_tensor("hm0", [2 * T, D], F32, kind="Internal").ap()
            h1 = nc.dram_tensor("h1", [2 * T, D], F32, kind="Internal").ap()
            hm1 = nc.dram_tensor("hm1", [T, D], F32, kind="Internal").ap()
            KT_all = nc.dram_tensor("KT_all", [128, 4, 2 * T], BF16, kind="Internal").ap()
            V_all = nc.dram_tensor("V_all", [32, 128, 512], BF16, kind="Internal").ap()
            km_all = nc.dram_tensor("km_all", [128, 4, 16], F32, kind="Internal").ap()
            gla_phase(c, x_cat, w_in, w_gate_up, b_gate, g_norm, cst["cmask"], aT_d, first_out=0)
            proj_ln_phase(c, aT_d, w_o_a, x_cat, ln_mix_g[0:1, :], ln_mix_b[0:1, :], hm0, ntiles=2 * T // 128)
            ffn_phase(c, hm0, [w_gu_dense], [w_down_dense], F_DENSE, ln_ffn_g[0:1, :], ln_ffn_b[0:1, :], h1, ntok=2 * T)
            kv_phase(c, h1, w_kv, KT_all, V_all, km_all, ntok=2 * T)
            h1o = h1[T:2 * T, :]
            moba_phase(c, h1o, w_q, KT_all, V_all, km_all, gbias, cst["cmask"], aT_d)
            proj_ln_phase(c, aT_d, w_o_b, h1o, ln_mix_g[1:2, :], ln_mix_b[1:2, :], hm1)
            ffn_phase(c, hm1, [w_gu_moe[e] for e in range(NEXP)], [w_down_moe[e] for e in range(NEXP)], F_EXP,
                      ln_ffn_g[1:2, :], ln_ffn_b[1:2, :], out, w_router=w_router, esel_d=esel)
        elif mode.startswith("T_"):
            x_own = dram_in(nc, "x_own", [T, D])
            if mode.startswith("T_kv"):
                w_kv = dram_in(nc, "w_kv", [D, 1024])
                KT_d = dram_out(nc, "KT_d", [128, 4, T], BF16); V_d = dram_out(nc, "V_d", [T // 128, 128, 512], BF16)
                km_d = dram_out(nc, "km_d", [128, 4, 8])
                kv_phase(c, x_own, w_kv, KT_d, V_d, km_d)
            if mode == "T_moe":
                esel = dram_in(nc, "esel", [8, NEXP, 128]); w_router = dram_in(nc, "w_router", [D, NEXP])
                w_gu_moe = dram_in(nc, "w_gu_moe", [1, D, 2 * F_EXP]); w_down_moe = dram_in(nc, "w_down_moe", [1, F_EXP, D])
                out = dram_out(nc, "out", [T, D])
                ffn_phase(c, x_own, [w_gu_moe[0]], [w_down_moe[0]], F_EXP, ln_ffn_g[1:2, :], ln_ffn_b[1:2, :], out, w_router=w_router, esel_d=esel)
            if mode == "T_ffn":
                w_gu_dense = dram_in(nc, "w_gu_dense", [D, 2 * F_DENSE]); w_down_dense = dram_in(nc, "w_down_dense", [F_DENSE, D])
                h1 = dram_out(nc, "h1", [T, D])
                ffn_phase(c, x_own, [w_gu_dense], [w_down_dense], F_DENSE, ln_ffn_g[0:1, :], ln_ffn_b[0:1, :], h1)
        elif mode in ("L1", "L1a", "L1b", "L1c"):
            x_cat = dram_in(nc, "x_cat", [2 * T, D]); x_own = x_cat[T:2 * T, :]
            w_in = dram_in(nc, "w_in", [D, 6160]); w_gate_up = dram_in(nc, "w_gate_up", [16, 1024])
            b_gate = dram_in(nc, "b_gate", [1, 1024]); g_norm = dram_in(nc, "g_norm", [1, 512])
            w_o_a = dram_in(nc, "w_o_a", [D, D])
            w_gu_dense = dram_in(nc, "w_gu_dense", [D, 2 * F_DENSE]); w_down_dense = dram_in(nc, "w_down_dense", [F_DENSE, D])
            w_kv = dram_in(nc, "w_kv", [D, 1024])
            hm0 = dram_out(nc, "hm0", [T, D]); h1 = dram_out(nc, "h1", [T, D])
            KT_d = dram_out(nc, "KT_d", [128, 4, T], BF16); V_d = dram_out(nc, "V_d", [T // 128, 128, 512], BF16)
            km_d = dram_out(nc, "km_d", [128, 4, 8])
            gla_phase(c, x_cat, w_in, w_gate_up, b_gate, g_norm, cst["cmask"], aT_d)
            proj_ln_phase(c, aT_d, w_o_a, x_own, ln_mix_g[0:1, :], ln_mix_b[0:1, :], hm0)
            if mode in ("L1", "L1b"):
                ffn_phase(c, hm0, [w_gu_dense], [w_down_dense], F_DENSE, ln_ffn_g[0:1, :], ln_ffn_b[0:1, :], h1)
            if mode in ("L1", "L1c"):
                kv_phase(c, h1 if mode == "L1" else hm0, w_kv, KT_d, V_d, km_d)
        else:
            h1 = dram_in(nc, "h1", [T, D])
            KT_all = dram_in(nc, "KT_all", [128, 4, 2 * T], BF16); V_all = dram_in(nc, "V_all", [32, 128, 512], BF16)
            km_all = dram_in(nc, "km_all", [128, 4, 16]); gbias = dram_in(nc, "gbias", [1, 128])
            esel = dram_in(nc, "esel", [8, NEXP, 128])
            w_q = dram_in(nc, "w_q", [D, D]); w_o_b = dram_in(nc, "w_o_b", [D, D])
            hm1 = dram_out(nc, "hm1", [T, D]); out = dram_out(nc, "out", [T, D])
            if mode == "L2":
                w_router = dram_in(nc, "w_router", [D, NEXP])
                w_gu_moe = dram_in(nc, "w_gu_moe", [NEXP, D, 2 * F_EXP]); w_down_moe = dram_in(nc, "w_down_moe", [NEXP, F_EXP, D])
            moba_phase(c, h1, w_q, KT_all, V_all, km_all, gbias, cst["cmask"], aT_d)
            proj_ln_phase(c, aT_d, w_o_b, h1, ln_mix_g[1:2, :], ln_mix_b[1:2, :], hm1)
            if mode == "L2":
                ffn_phase(c, hm1, [w_gu_moe[e] for e in range(NEXP)], [w_down_moe[e] for e in range(NEXP)], F_EXP,
                          ln_ffn_g[1:2, :], ln_ffn_b[1:2, :], out, w_router=w_router, esel_d=esel)
        p.emit()
    return nc


_NC_CACHE = {}


def _get_nc(mode):
    if mode not in _NC_CACHE:
        _NC_CACHE[mode] = build(mode)
    return _NC_CACHE[mode]


def kernel(x, w_in_a, w_gate_up_a, b_gate_a, g_norm_a, w_o_a, w_kv_shared, w_q_b, w_o_b,
           ln_mix_g, ln_mix_b, w_gu_dense, w_down_dense, w_router, w_gu_moe, w_down_moe,
           ln_ffn_g, ln_ffn_b):
    f = lambda a: np.ascontiguousarray(np.asarray(a, dtype=np.float32))
    x = f(x)
    esel = np.zeros((8, NEXP, 128), np.float32)
    for e in range(NEXP):
        esel[e, e, :] = 1.0
    shared = {"ident": np.eye(128, dtype=np.float32), "cmask": np.triu(np.ones((128, 128), np.float32)),
              "ln_mix_g": f(ln_mix_g), "ln_mix_b": f(ln_mix_b), "ln_ffn_g": f(ln_ffn_g), "ln_ffn_b": f(ln_ffn_b),
              "w_in": f(w_in_a[0]), "w_gate_up": f(w_gate_up_a[0]), "b_gate": f(b_gate_a).reshape(1, 1024),
              "g_norm": f(g_norm_a).reshape(1, 512), "w_o_a": f(w_o_a[0]), "w_gu_dense": f(w_gu_dense[0]),
              "w_down_dense": f(w_down_dense[0]), "w_kv": f(w_kv_shared),
              "w_q": f(w_q_b[0]), "w_o_b": f(w_o_b[0]), "w_router": f(w_router[0]), "w_gu_moe": f(w_gu_moe[0]),
              "w_down_moe": f(w_down_moe[0]), "esel": esel}
    ins = []
    for core in range(8):
        b, half = core // 2, core % 2
        xc = np.zeros((2 * T, D), np.float32)
        if half == 1:
            xc[0:T] = x[b, 0:T]
        xc[T:2 * T] = x[b, half * T:(half + 1) * T]
        gb = np.full((8, 16), -1e30, np.float32)
        for qb in range(8):
            if half == 1:
                gb[qb, 0:8] = 0.0
            gb[qb, 8:8 + qb] = 0.0
        d = {"x_cat": xc, "gbias": gb.reshape(1, 128)}
        d.update(shared)
        ins.append(d)
    res = run_bass_kernel_spmd(_get_nc("F"), ins, core_ids=list(range(8))).results
    out = np.empty((4, 2 * T, D), np.float32)
    for core in range(8):
        b, half = core // 2, core % 2
        out[b, half * T:(half + 1) * T] = res[core]["out"]
    return out
```
